# Optimizing a Trainium2 kernel written in Bass

```python
import jax, jax.numpy as jnp
from jax import lax
import numpy as np

D_MODEL = 2048
BATCH = 8
SEQ = 4096
DEPTH = 2

CHUNK = 64
Q_BLOCK = 128
D_PLE = 256
D_FF = 4 * D_MODEL
LN_EPS = 1e-5
ATT_HEADS = 8
ATT_HEAD_DIM = 128
ATT_WIDTH = ATT_HEADS * ATT_HEAD_DIM
CONV_CH = D_MODEL - ATT_WIDTH
CONV_WIDTH = 31
EVEN_IN = 3 * ATT_WIDTH + ATT_HEADS + 2 * CONV_CH
ML_HEADS = 8
ML_QK_DIM = 128
ML_V_DIM = D_MODEL // ML_HEADS
ML_QK_WIDTH = ML_HEADS * ML_QK_DIM
ML_V_WIDTH = ML_HEADS * ML_V_DIM
ODD_IN = 2 * ML_QK_WIDTH + 2 * ML_V_WIDTH + 2 * ML_HEADS
N_EVEN = (DEPTH + 1) // 2
N_ODD = DEPTH // 2
ALPHA = (2 * DEPTH) ** 0.25
BETA = (8 * DEPTH) ** -0.25

kernel_name = "hybrid_fox_conformer_mlstm_deepnorm"


def layer_norm(x, g, b):
    xf = x.astype(jnp.float32)
    mu = jnp.mean(xf, axis=-1, keepdims=True)
    xc = xf - mu
    var = jnp.mean(jnp.square(xc), axis=-1, keepdims=True)
    y = xc * lax.rsqrt(var + LN_EPS) * g.astype(jnp.float32) + b.astype(jnp.float32)
    return y.astype(x.dtype)


def split_heads(t, n_heads):
    B, S, W = t.shape
    return t.reshape(B, S, n_heads, W // n_heads).transpose(0, 2, 1, 3)


def forgetting_attention(q, k, v, f_logit):
    B, H, S, Dh = q.shape
    nb = S // Q_BLOCK
    scale = Dh ** -0.5
    log_f = jax.nn.log_sigmoid(f_logit.astype(jnp.float32))
    F = jnp.cumsum(log_f, axis=1).transpose(0, 2, 1)
    qb = jnp.moveaxis(q.reshape(B, H, nb, Q_BLOCK, Dh), 2, 0)
    Fq = jnp.moveaxis(F.reshape(B, H, nb, Q_BLOCK), 2, 0)
    q_pos = jnp.arange(S, dtype=jnp.int32).reshape(nb, Q_BLOCK)
    k_pos = jnp.arange(S, dtype=jnp.int32)

    def block(args):
        qi, Fi, pi = args
        s = jnp.einsum('bhqd,bhkd->bhqk', qi, k, preferred_element_type=jnp.float32) * scale
        s = s + Fi[..., :, None] - F[:, :, None, :]
        s = jnp.where(k_pos[None, :] <= pi[:, None], s, -jnp.inf)
        pr = jax.nn.softmax(s, axis=-1)
        return jnp.einsum('bhqk,bhkd->bhqd', pr.astype(v.dtype), v)

    out = lax.map(block, (qb, Fq, q_pos))
    return out.transpose(1, 0, 3, 2, 4).reshape(B, S, H * Dh)


def conformer_conv(u, dw_kernel, dw_bias, norm_g, norm_b):
    a, gate = jnp.split(u, 2, axis=-1)
    y = a * jax.nn.sigmoid(gate)
    C = y.shape[-1]
    y = lax.conv_general_dilated(
        y, dw_kernel[:, None, :].astype(y.dtype), window_strides=(1,),
        padding=((CONV_WIDTH - 1, 0),), dimension_numbers=('NWC', 'WIO', 'NWC'),
        feature_group_count=C) + dw_bias.astype(y.dtype)
    y = layer_norm(y, norm_g, norm_b)
    return jax.nn.silu(y)


def mlstm_chunkwise(q, k, v, i_logit, f_logit):
    B, H, S, dk = q.shape
    dv = v.shape[-1]
    nc = S // CHUNK
    f32 = jnp.float32
    q = q.astype(f32)
    k = k.astype(f32) * (dk ** -0.5)
    v = v.astype(f32)
    log_f = jax.nn.log_sigmoid(f_logit.astype(f32))
    i_pre = i_logit.astype(f32)

    def to_chunks(t):
        return jnp.moveaxis(t.reshape(B, H, nc, CHUNK, *t.shape[3:]), 2, 0)

    tri = jnp.tril(jnp.ones((CHUNK, CHUNK), dtype=bool))

    def step(carry, xs):
        C, n, m = carry
        qc, kc, vc, ic, fc = xs
        b = jnp.cumsum(fc, axis=-1)
        d_intra = jnp.where(tri, b[..., :, None] - b[..., None, :] + ic[..., None, :], -jnp.inf)
        d_inter = b + m[..., None]
        m_t = jnp.maximum(d_inter, jnp.max(d_intra, axis=-1))
        w_intra = jnp.exp(d_intra - m_t[..., None])
        w_inter = jnp.exp(d_inter - m_t)
        s = jnp.einsum('bhtd,bhsd->bhts', qc, kc) * w_intra
        num = (w_inter[..., None] * jnp.einsum('bhtd,bhde->bhte', qc, C)
               + jnp.einsum('bhts,bhse->bhte', s, vc))
        den = w_inter * jnp.einsum('bhtd,bhd->bht', qc, n) + jnp.sum(s, axis=-1)
        h = num / jnp.maximum(jnp.abs(den), jnp.exp(-m_t))[..., None]
        b_last = b[..., -1]
        g = b_last[..., None] - b + ic
        m_new = jnp.maximum(b_last + m, jnp.max(g, axis=-1))
        decay = jnp.exp(b_last + m - m_new)
        wk = jnp.exp(g - m_new[..., None])[..., None] * kc
        C_new = decay[..., None, None] * C + jnp.einsum('bhsd,bhse->bhde', wk, vc)
        n_new = decay[..., None] * n + jnp.sum(wk, axis=2)
        return (C_new, n_new, m_new), h

    init = (jnp.zeros((B, H, dk, dv), f32), jnp.zeros((B, H, dk), f32), jnp.zeros((B, H), f32))
    xs = (to_chunks(q), to_chunks(k), to_chunks(v), to_chunks(i_pre), to_chunks(log_f))
    _, h = lax.scan(step, init, xs)
    return jnp.moveaxis(h, 0, 2).reshape(B, H, S, dv).transpose(0, 2, 1, 3).reshape(B, S, H * dv)


def even_mixer(x, w_in, b_fgate, dw_kernel, dw_bias, cnorm_g, cnorm_b, w_out):
    z = x @ w_in
    q, k, v, f_pre, u = jnp.split(
        z, (ATT_WIDTH, 2 * ATT_WIDTH, 3 * ATT_WIDTH, 3 * ATT_WIDTH + ATT_HEADS), axis=-1)
    attn = forgetting_attention(split_heads(q, ATT_HEADS), split_heads(k, ATT_HEADS),
                                split_heads(v, ATT_HEADS), f_pre + b_fgate)
    conv = conformer_conv(u, dw_kernel, dw_bias, cnorm_g, cnorm_b)
    return jnp.concatenate([attn.astype(x.dtype), conv.astype(x.dtype)], axis=-1) @ w_out


def odd_mixer(x, w_in, b_igate, b_fgate, w_out):
    z = x @ w_in
    o1 = 2 * ML_QK_WIDTH + ML_V_WIDTH
    q, k, v, i_pre, f_pre, o_pre = jnp.split(
        z, (ML_QK_WIDTH, 2 * ML_QK_WIDTH, o1, o1 + ML_HEADS, o1 + 2 * ML_HEADS), axis=-1)
    h = mlstm_chunkwise(split_heads(q, ML_HEADS), split_heads(k, ML_HEADS), split_heads(v, ML_HEADS),
                        (i_pre + b_igate).transpose(0, 2, 1), (f_pre + b_fgate).transpose(0, 2, 1))
    return (jax.nn.sigmoid(o_pre) * h.astype(x.dtype)) @ w_out


def setup_inputs(seed: int = 0) -> dict:
    key = jax.random.key(seed)
    ks = jax.random.split(key, 24)
    nrm = jax.random.normal
    f32 = jnp.float32
    D = D_MODEL
    return {
        "x": nrm(ks[0], (BATCH, SEQ, D), f32),
        "p": nrm(ks[1], (DEPTH, BATCH, SEQ, D_PLE), f32),
        "ev_w_in": nrm(ks[2], (N_EVEN, D, EVEN_IN), f32) * D ** -0.5,
        "ev_b_fgate": 2.0 + 0.1 * nrm(ks[3], (N_EVEN, ATT_HEADS), f32),
        "ev_dw_kernel": nrm(ks[4], (N_EVEN, CONV_WIDTH, CONV_CH), f32) * CONV_WIDTH ** -0.5,
        "ev_dw_bias": 0.02 * nrm(ks[5], (N_EVEN, CONV_CH), f32),
        "ev_cnorm_g": 1.0 + 0.02 * nrm(ks[6], (N_EVEN, CONV_CH), f32),
        "ev_cnorm_b": 0.02 * nrm(ks[7], (N_EVEN, CONV_CH), f32),
        "ev_w_out": nrm(ks[8], (N_EVEN, D, D), f32) * (D ** -0.5 * BETA),
        "od_w_in": nrm(ks[9], (N_ODD, D, ODD_IN), f32) * D ** -0.5,
        "od_b_igate": 0.1 * nrm(ks[10], (N_ODD, ML_HEADS), f32),
        "od_b_fgate": 3.0 + 0.1 * nrm(ks[11], (N_ODD, ML_HEADS), f32),
        "od_w_out": nrm(ks[12], (N_ODD, D, D), f32) * (D ** -0.5 * BETA),
        "ln_mix_g": 1.0 + 0.02 * nrm(ks[13], (DEPTH, D), f32),
        "ln_mix_b": 0.02 * nrm(ks[14], (DEPTH, D), f32),
        "w_up": nrm(ks[15], (DEPTH, D, D_FF), f32) * D ** -0.5,
        "w_down": nrm(ks[16], (DEPTH, D_FF, D), f32) * (D_FF ** -0.5 * BETA),
        "ln_ffn_g": 1.0 + 0.02 * nrm(ks[17], (DEPTH, D), f32),
        "ln_ffn_b": 0.02 * nrm(ks[18], (DEPTH, D), f32),
        "w_ple": nrm(ks[19], (DEPTH, D_PLE, D), f32) * D_PLE ** -0.5,
        "w_ple_gate": nrm(ks[20], (DEPTH, D, D), f32) * D ** -0.5,
    }


def reference(x, p, ev_w_in, ev_b_fgate, ev_dw_kernel, ev_dw_bias, ev_cnorm_g, ev_cnorm_b, ev_w_out,
              od_w_in, od_b_igate, od_b_fgate, od_w_out, ln_mix_g, ln_mix_b, w_up, w_down,
              ln_ffn_g, ln_ffn_b, w_ple, w_ple_gate):
    for i in range(DEPTH):
        j = i // 2
        if i % 2 == 0:
            y = even_mixer(x, ev_w_in[j], ev_b_fgate[j], ev_dw_kernel[j], ev_dw_bias[j],
                           ev_cnorm_g[j], ev_cnorm_b[j], ev_w_out[j])
        else:
            y = odd_mixer(x, od_w_in[j], od_b_igate[j], od_b_fgate[j], od_w_out[j])
        x = layer_norm(ALPHA * x + y, ln_mix_g[i], ln_mix_b[i])
        hid = jnp.square(jax.nn.relu(x @ w_up[i]))
        x = layer_norm(ALPHA * x + hid @ w_down[i], ln_ffn_g[i], ln_ffn_b[i])
        x = x + jax.nn.sigmoid(x @ w_ple_gate[i]) * (p[i] @ w_ple[i])
    return x
```

```python
import contextlib
from contextlib import ExitStack
import numpy as np
import ml_dtypes
import concourse.bass as bass
import concourse.mybir as mybir
from concourse.bass_utils import run_bass_kernel_spmd

F32 = mybir.dt.float32
BF16 = mybir.dt.bfloat16
AF = mybir.ActivationFunctionType
ALU = mybir.AluOpType

ENGS = ("pe", "act", "dve", "pool", "sp")


class Buf:
    __slots__ = ("name", "last_w", "readers")

    def __init__(self, name):
        self.name = name
        self.last_w = None
        self.readers = []


class DSem:
    __slots__ = ("name", "count", "h")

    def __init__(self, name):
        self.name = name
        self.count = 0
        self.h = None


class Op:
    __slots__ = ("eng", "fn", "deps", "needs_inc", "tokval", "dsem", "is_dma", "idx")

    def __init__(self, eng, fn, is_dma=False, dsem=None):
        self.eng = eng
        self.fn = fn
        self.deps = []
        self.needs_inc = False
        self.tokval = None
        self.dsem = dsem
        self.is_dma = is_dma
        self.idx = None


class _Rec:
    def __init__(self):
        self.call = None

    def __getattr__(self, name):
        def f(*a, **kw):
            self.call = (name, a, kw)
        return f


class Prog:
    def __init__(self):
        self.ops = {e: [] for e in ENGS}
        self.dsems = []
        self.free_dsems = []
        self.phase_dsems = []
        self.nbuf = 0
        self.last = {e: None for e in ENGS}
        self.pending_dma = []
        self.nops = 0

    def buf(self, name=None):
        self.nbuf += 1
        return Buf(name or f"b{self.nbuf}")

    def dsem(self, name=None):
        if self.free_dsems:
            d = self.free_dsems.pop()
        else:
            d = DSem(name or f"d{len(self.dsems)}")
            self.dsems.append(d)
        self.phase_dsems.append(d)
        return d

    def _add(self, op, reads, writes, pe_accum=False):
        deps = []
        for b in reads:
            if b.last_w is not None:
                deps.append(b.last_w)
        for b in writes:
            if b.last_w is not None:
                lw = b.last_w
                if not (pe_accum and lw.eng == "pe" and op.eng == "pe" and not lw.is_dma and not op.is_dma):
                    deps.append(lw)
            deps.extend(b.readers)
        seen = set()
        for d in deps:
            if d is op or id(d) in seen:
                continue
            seen.add(id(d))
            op.deps.append(d)
            d.needs_inc = True
        for b in reads:
            b.readers.append(op)
        for b in writes:
            b.last_w = op
            b.readers = []
        op.idx = self.nops
        self.nops += 1
        self.ops[op.eng].append(op)
        if op.is_dma:
            self.pending_dma.append(op)
        else:
            self.last[op.eng] = op
        return op

    @staticmethod
    def _freeze(fn):
        rec = _Rec()
        fn(rec)
        name, a, kw = rec.call
        return lambda h: getattr(h, name)(*a, **kw)

    def op(self, eng, fn, reads=(), writes=(), pe_accum=False):
        return self._add(Op(eng, self._freeze(fn)), list(reads), list(writes), pe_accum)

    def dma(self, queue, fn, dsem, reads=(), writes=(), after=()):
        op = Op(queue, self._freeze(fn), is_dma=True, dsem=dsem)
        dsem.count += 16
        op.tokval = dsem.count
        for b in after:
            if b.last_w is not None and b.last_w not in op.deps:
                op.deps.append(b.last_w)
                b.last_w.needs_inc = True
        return self._add(op, list(reads), list(writes))

    def barrier(self):
        deps = [o for o in self.last.values() if o is not None] + list(self.pending_dma)
        for e in ENGS:
            op = Op(e, None)
            for d in deps:
                op.deps.append(d)
                d.needs_inc = True
            op.idx = self.nops
            self.nops += 1
            self.ops[e].append(op)
        self.pending_dma = []
        self.free_dsems.extend(self.phase_dsems)
        self.phase_dsems = []

    def emit(self, nc, final_wait_ops=()):
        engh = {"pe": nc.tensor, "act": nc.scalar, "dve": nc.vector, "pool": nc.gpsimd, "sp": nc.sync}
        for e in ENGS:
            c = 0
            for op in self.ops[e]:
                if not op.is_dma and op.needs_inc and op.fn is not None:
                    c += 1
                    op.tokval = c
        with ExitStack() as st:
            esem = {e: st.enter_context(nc.semaphore(f"s_{e}")) for e in ENGS}
            for d in self.dsems:
                if d.count > 0:
                    d.h = st.enter_context(nc.semaphore(f"q_{d.name}"))
            nwait = 0
            for e in ENGS:
                h = engh[e]
                known = {}
                for op in self.ops[e]:
                    need = {}
                    for d in op.deps:
                        if d.is_dma:
                            key = ("d", id(d.dsem))
                            semh = d.dsem.h
                        else:
                            key = ("e", d.eng)
                            semh = esem[d.eng]
                        v = d.tokval
                        if known.get(key, 0) >= v:
                            continue
                        if key not in need or need[key][1] < v:
                            need[key] = (semh, v)
                    for key, (semh, v) in need.items():
                        h.wait_ge(semh, v)
                        known[key] = v
                        nwait += 1
                    if op.fn is None:
                        continue
                    ins = op.fn(h)
                    if op.is_dma:
                        ins.then_inc(op.dsem.h, 16)
                    elif op.needs_inc:
                        ins.then_inc(esem[e], 1)
            for op in final_wait_ops:
                if op.is_dma:
                    nc.sync.wait_ge(op.dsem.h, op.tokval)
                else:
                    nc.sync.wait_ge(esem[op.eng], op.tokval)
            self.nwait = nwait


S = 4096
D = 2048
NT = S // 128
NB = S // 512
DFF = 8192
ALPHA = float(4 ** 0.25)
EPS = 1e-5
EV_IN = 5128
OD_IN = 6160

WEIGHTS = [
    ("ev_w_in", (D, EV_IN)), ("ev_w_out", (D, D)), ("od_w_in", (D, OD_IN)), ("od_w_out", (D, D)),
]


class Ring:
    def __init__(self, kb, name, n, shape, dt):
        self.t = [kb.sb(f"{name}{i}", shape, dt) for i in range(n)]
        self.b = [kb.P.buf(f"{name}{i}") for i in range(n)]
        self.ld = [kb.P.dsem() for i in range(n)]
        self.st = [kb.P.dsem() for i in range(n)]
        self.i = -1
        self.n = n

    def next(self):
        self.i += 1
        j = self.i % self.n
        return self.t[j], self.b[j], self.ld[j], self.st[j]


class KB:
    def __init__(self, dbg=()):
        self.nc = bass.Bass("TRN2", target_bir_lowering=False)
        self.P = Prog()
        self.dbg = set(dbg)
        self.ap = {}
        self.db = {}
        self.uid = 0
        self.st = None
        self.flip = 0
        self.cq = []
        self.cpending = {}

    def din(self, name, shape, dt=F32):
        self.ap[name] = self.nc.dram_tensor(name, list(shape), dt, kind="ExternalInput").ap()
        return self.ap[name]

    def dscr(self, name, shape, dt, nbuf=NB, out=False):
        kind = "ExternalOutput" if (out or name in self.dbg) else "Internal"
        self.ap[name] = self.nc.dram_tensor(name, list(shape), dt, kind=kind).ap()
        self.db[name] = [self.P.buf(f"{name}.{i}") for i in range(nbuf)]
        return self.ap[name]

    def sb(self, name, shape, dt):
        self.uid += 1
        return self.st.enter_context(self.nc.sbuf_tensor(f"{name}_{self.uid}", list(shape), dt))

    @contextlib.contextmanager
    def phase(self):
        with ExitStack() as st:
            self.st = st
            self.uid += 1
            self.ps = [st.enter_context(self.nc.psum_tensor(f"ps{i}_{self.uid}", [128, 512], F32)) for i in range(8)]
            self.pb = [self.P.buf(f"ps{i}") for i in range(8)]
            self.psi = -1
            yield
            self.P.barrier()
        self.st = None

    def bank(self, lo=0, hi=8):
        self.psi += 1
        j = lo + self.psi % (hi - lo)
        return self.ps[j], self.pb[j]

    def alt(self):
        self.flip ^= 1
        return self.flip


def convert_weight(kb, src_ap, name, shape):
    P = kb.P
    n = int(np.prod(shape))
    C = 2048
    rows = n // C
    assert rows * C == n
    dst = kb.nc.dram_tensor(name, list(shape), BF16, kind="Internal").ap()
    kb.ap[name] = dst
    sf = src_ap.rearrange("k n -> (k n)").rearrange("(a c) -> a c", c=C)
    df = dst.rearrange("k n -> (k n)").rearrange("(a c) -> a c", c=C)
    ds = P.dsem()
    P.phase_dsems.remove(ds)
    kb.db[name] = []
    kb.cpending[name] = 0
    R = 256
    for r0 in range(0, rows, R):
        r1 = min(rows, r0 + R)
        kb.cq.append((name, sf[r0:r1, :], df[r0:r1, :], ds))
        kb.cpending[name] += 1


def pump(kb, n=None, gate=(), upto=None):
    P = kb.P
    k = 0
    while kb.cq:
        if upto is not None:
            if kb.cpending.get(upto, 0) == 0:
                break
        elif n is not None and k >= n:
            break
        name, s_, d_, ds = kb.cq.pop(0)
        b = P.buf()
        P.dma("pool", lambda e: e.dma_start(out=d_, in_=s_), ds, after=list(gate), writes=[b])
        kb.db[name].append(b)
        kb.cpending[name] -= 1
        k += 1


def need_w(kb, name):
    if kb.cpending.get(name, 0) > 0:
        pump(kb, upto=name)
    return kb.db[name]


def cast_tile(kb, src_t, src_b, W, xb_ring, cast_eng=None):
    P = kb.P
    xb, xb_b, _, _ = xb_ring.next()
    ce = cast_eng or ("act" if kb.alt() else "dve")
    if ce == "act":
        P.op("act", lambda e: e.copy(out=xb[:, :W], in_=src_t), reads=[src_b], writes=[xb_b])
    elif ce == "pool":
        P.op("pool", lambda e: e.tensor_copy(out=xb[:, :W], in_=src_t), reads=[src_b], writes=[xb_b])
    else:
        P.op("dve", lambda e: e.tensor_copy(out=xb[:, :W], in_=src_t), reads=[src_b], writes=[xb_b])
    return xb, xb_b


def transpose_tile(kb, xb, xb_b, W, dstT, dstT_b, col0, ident, ident_b):
    P = kb.P
    nk = W // 128
    for g0 in range(0, nk, 4):
        ng = min(4, nk - g0)
        ps, psb = kb.bank(0, 2)
        pv = ps.bitcast(BF16)
        for a in range(ng):
            kc = g0 + a
            P.op("pe", lambda e, a=a, kc=kc, pv=pv: e.transpose(out=pv[:, 128 * a:128 * (a + 1)], in_=xb[:, 128 * kc:128 * (kc + 1)], identity=ident),
                 reads=[xb_b, ident_b], writes=[psb], pe_accum=True)
        src = pv[:, 0:128 * ng].rearrange("p (a c) -> p a c", c=128)
        dst = dstT[:, g0:g0 + ng, col0:col0 + 128]
        if kb.alt():
            P.op("act", lambda e, src=src, dst=dst: e.copy(out=dst, in_=src), reads=[psb], writes=[dstT_b])
        else:
            P.op("dve", lambda e, src=src, dst=dst: e.tensor_copy(out=dst, in_=src), reads=[psb], writes=[dstT_b])


def cast_transpose(kb, src_t, src_b, W, xb_ring, dstT, dstT_b, col0, ident, ident_b, cast_eng=None):
    xb, xb_b = cast_tile(kb, src_t, src_b, W, xb_ring, cast_eng)
    transpose_tile(kb, xb, xb_b, W, dstT, dstT_b, col0, ident, ident_b)


def load_consts(kb):
    P = kb.P
    c = {}
    for nm, dt in (("ident_bf", BF16), ("tri_bf", BF16), ("ones_bf", BF16), ("tri_f", F32), ("ones_f", F32), ("ident_f", F32)):
        t = kb.sb(nm, [128, 128], dt)
        b = P.buf(nm)
        ds = P.dsem()
        P.dma("sp", lambda e, t=t, nm=nm: e.dma_start(out=t[:], in_=kb.ap[nm]), ds, writes=[b])
        c[nm] = (t, b)
    return c


def phase_A(kb, layer, xsrc):
    P, nc = kb.P, kb.nc
    ev = layer == 0
    wname = "wb_ev_in" if ev else "wb_od_in"
    NIN = EV_IN if ev else OD_IN
    wv = kb.ap[wname].rearrange("(kc p) n -> p kc n", p=128)
    wbufs = need_w(kb, wname)
    ng = 8 if ev else 16
    gcol = 3072 if ev else 4096
    with kb.phase():
        C = load_consts(kb)
        ident, ident_b = C["ident_bf"]
        xs_r = Ring(kb, "xs", 2, [128, D], F32)
        xb_r = Ring(kb, "xb", 2, [128, D], BF16)
        xT_r = Ring(kb, "xT", 2, [128, 16, 512], BF16)
        w_r = Ring(kb, "wst", 3, [128, 16, 512], BF16)
        o16_r = Ring(kb, "o16", 3, [128, 4, 512], BF16)
        o32_r = Ring(kb, "o32", 2, [128, 4, 512], F32)
        sg_r = Ring(kb, "sg", 2, [128, 512], F32)
        wg = kb.sb("wg", [128, 16, 16], BF16)
        wg_b = P.buf("wg")
        gt = kb.sb("gt", [128, NT, 16], F32)
        gt_b = P.buf("gt")
        gbias = kb.sb("gbias", [128, 16], F32)
        gbias_b = P.buf("gbias")
        P.dma("sp", lambda e: e.dma_start(out=wg[:, :, :ng], in_=wv[:, :, gcol:gcol + ng]), P.dsem(), reads=wbufs, writes=[wg_b])
        if ev:
            P.dma("sp", lambda e: e.dma_start(out=gbias[:, 0:8], in_=kb.ap["ev_b_fgate"][0:1, :].partition_broadcast(128)), P.dsem(), writes=[gbias_b])
        else:
            d_ = P.dsem()
            P.dma("sp", lambda e: e.dma_start(out=gbias[:, 0:8], in_=kb.ap["od_b_igate"][0:1, :].partition_broadcast(128)), d_, writes=[gbias_b])
            P.dma("sp", lambda e: e.dma_start(out=gbias[:, 8:16], in_=kb.ap["od_b_fgate"][0:1, :].partition_broadcast(128)), d_, writes=[gbias_b])

        if ev:
            groups = [("fm", "qT", 0, 0), ("fm", "qT", 512, 4), ("fm", "kT", 1024, 0), ("fm", "kT", 1536, 4),
                      ("tm", "v", 2048, 0), ("tm", "v", 2560, 512),
                      ("glu", "g", 3080, 0), ("glu", "g", 3080 + 512, 4)]
        else:
            groups = [("fm", "qT", 0, 0), ("fm", "qT", 512, 4), ("fm", "kT", 1024, 0), ("fm", "kT", 1536, 4)]
            groups += [("tm", "v", 2048 + 512 * i, 512 * i) for i in range(4)]
            groups += [("sig", "oT", 4112 + 512 * i, 4 * i) for i in range(4)]

        def load_w(c0, width=512):
            wt, wb_, wl, _ = w_r.next()
            P.dma("sp", lambda e: e.dma_start(out=wt[:, :, :width], in_=wv[:, :, c0:c0 + width]), wl, reads=wbufs, writes=[wb_])
            return wt, wb_

        for tb in range(NB):
            xT, xT_b, _, _ = xT_r.next()
            for t in range(4):
                xs, xs_b, xl, _ = xs_r.next()
                r0 = tb * 512 + t * 128
                P.dma("sp", lambda e, xs=xs, r0=r0: e.dma_start(out=xs[:], in_=xsrc[0][r0:r0 + 128, :]), xl, reads=[xsrc[1][tb]], writes=[xs_b])
                cast_transpose(kb, xs[:], xs_b, D, xb_r, xT, xT_b, 128 * t, ident[:], ident_b)
            pump(kb, n=3 if ev else 0, gate=[xT_b])
            for t in range(4):
                ps, psb = kb.bank(2, 8)
                for kc in range(16):
                    P.op("pe", lambda e, ps=ps, kc=kc, t=t, xT=xT: e.matmul(ps[:, 0:ng], lhsT=xT[:, kc, 128 * t:128 * (t + 1)], rhs=wg[:, kc, 0:ng], start=(kc == 0), stop=(kc == 15)),
                         reads=[xT_b, wg_b], writes=[psb], pe_accum=True)
                tt = tb * 4 + t
                P.op("dve", lambda e, ps=ps, tt=tt: e.tensor_tensor(out=gt[:, tt, 0:ng], in0=ps[:, 0:ng], in1=gbias[:, 0:ng], op=ALU.add),
                     reads=[psb, gbias_b], writes=[gt_b])
            for kind, dst, c0, d0 in groups:
                if kind == "fm" or kind == "sig":
                    wt, wb_ = load_w(c0)
                    o, o_b, _, ost = o16_r.next()
                    for m in range(4):
                        ps, psb = kb.bank(2, 8)
                        for kc in range(16):
                            P.op("pe", lambda e, ps=ps, kc=kc, m=m, wt=wt, xT=xT: e.matmul(ps[:], lhsT=wt[:, kc, 128 * m:128 * (m + 1)], rhs=xT[:, kc, :], start=(kc == 0), stop=(kc == 15)),
                                 reads=[xT_b, wb_], writes=[psb], pe_accum=True)
                        if kind == "sig":
                            P.op("act", lambda e, ps=ps, o=o, m=m: e.activation(out=o[:, m, :], in_=ps[:], func=AF.Sigmoid), reads=[psb], writes=[o_b])
                        elif dst == "kT" and not ev:
                            P.op("act", lambda e, ps=ps, o=o, m=m: e.mul(out=o[:, m, :], in_=ps[:], mul=float(128 ** -0.5)), reads=[psb], writes=[o_b])
                        elif kb.alt():
                            P.op("act", lambda e, ps=ps, o=o, m=m: e.copy(out=o[:, m, :], in_=ps[:]), reads=[psb], writes=[o_b])
                        else:
                            P.op("dve", lambda e, ps=ps, o=o, m=m: e.tensor_copy(out=o[:, m, :], in_=ps[:]), reads=[psb], writes=[o_b])
                    dap = kb.ap[dst][d0:d0 + 4, :, tb * 512:(tb + 1) * 512].rearrange("h p t -> p h t")
                    P.dma("act", lambda e, o=o, dap=dap: e.dma_start(out=dap, in_=o[:]), ost, reads=[o_b], writes=[kb.db[dst][tb]])
                elif kind == "tm":
                    wt, wb_ = load_w(c0)
                    o, o_b, _, ost = o16_r.next()
                    for t in range(4):
                        ps, psb = kb.bank(2, 8)
                        for kc in range(16):
                            P.op("pe", lambda e, ps=ps, kc=kc, t=t, wt=wt, xT=xT: e.matmul(ps[:], lhsT=xT[:, kc, 128 * t:128 * (t + 1)], rhs=wt[:, kc, :], start=(kc == 0), stop=(kc == 15)),
                                 reads=[xT_b, wb_], writes=[psb], pe_accum=True)
                        if kb.alt():
                            P.op("act", lambda e, ps=ps, o=o, t=t: e.copy(out=o[:, t, :], in_=ps[:]), reads=[psb], writes=[o_b])
                        else:
                            P.op("dve", lambda e, ps=ps, o=o, t=t: e.tensor_copy(out=o[:, t, :], in_=ps[:]), reads=[psb], writes=[o_b])
                    dap = kb.ap[dst][tb * 512:(tb + 1) * 512, d0:d0 + 512].rearrange("(t p) c -> p t c", p=128)
                    P.dma("act", lambda e, o=o, dap=dap: e.dma_start(out=dap, in_=o[:]), ost, reads=[o_b], writes=[kb.db[dst][tb]])
                elif kind == "glu":
                    wa, wa_b = load_w(c0)
                    wgt, wgt_b = load_w(c0 + 1024)
                    o, o_b, _, ost = o16_r.next()
                    for m in range(4):
                        pa, pa_b = kb.bank(2, 8)
                        pg, pg_b = kb.bank(2, 8)
                        for kc in range(16):
                            P.op("pe", lambda e, pg=pg, kc=kc, m=m, wgt=wgt, xT=xT: e.matmul(pg[:], lhsT=wgt[:, kc, 128 * m:128 * (m + 1)], rhs=xT[:, kc, :], start=(kc == 0), stop=(kc == 15)),
                                 reads=[xT_b, wgt_b], writes=[pg_b], pe_accum=True)
                        for kc in range(16):
                            P.op("pe", lambda e, pa=pa, kc=kc, m=m, wa=wa, xT=xT: e.matmul(pa[:], lhsT=wa[:, kc, 128 * m:128 * (m + 1)], rhs=xT[:, kc, :], start=(kc == 0), stop=(kc == 15)),
                                 reads=[xT_b, wa_b], writes=[pa_b], pe_accum=True)
                        sg, sg_b, _, _ = sg_r.next()
                        P.op("act", lambda e, pg=pg, sg=sg: e.activation(out=sg[:], in_=pg[:], func=AF.Sigmoid), reads=[pg_b], writes=[sg_b])
                        P.op("dve", lambda e, pa=pa, sg=sg, o=o, m=m: e.tensor_tensor(out=o[:, m, :], in0=pa[:], in1=sg[:], op=ALU.mult), reads=[pa_b, sg_b], writes=[o_b])
                    dap = kb.ap["g"][d0:d0 + 4, :, tb * 512:(tb + 1) * 512].rearrange("h p t -> p h t")
                    P.dma("act", lambda e, o=o, dap=dap: e.dma_start(out=dap, in_=o[:]), ost, reads=[o_b], writes=[kb.db["g"][tb]])
        P.dma("act", lambda e: e.dma_start(out=kb.ap["gates"].rearrange("(t p) g -> p t g", p=128), in_=gt[:]), P.dsem(), reads=[gt_b], writes=kb.db["gates"])


def load_cw(kb, C, lo=6, hi=8):
    P = kb.P
    cwr = kb.sb("cwr", [34, 1024], F32)
    cwr_b = P.buf("cwr")
    ds = P.dsem()
    P.dma("sp", lambda e: e.dma_start(out=cwr[0:31, :], in_=kb.ap["ev_dw_kernel"][0]), ds, writes=[cwr_b])
    P.dma("sp", lambda e: e.dma_start(out=cwr[31:32, :], in_=kb.ap["ev_dw_bias"][0:1, :]), ds, writes=[cwr_b])
    P.dma("sp", lambda e: e.dma_start(out=cwr[32:33, :], in_=kb.ap["ev_cnorm_g"][0:1, :]), ds, writes=[cwr_b])
    P.dma("sp", lambda e: e.dma_start(out=cwr[33:34, :], in_=kb.ap["ev_cnorm_b"][0:1, :]), ds, writes=[cwr_b])
    cw = kb.sb("cw", [128, 8, 34], F32)
    cw_b = P.buf("cw")
    idf, idf_b = C["ident_f"]
    for c in range(8):
        ps, psb = kb.bank(lo, hi)
        P.op("pe", lambda e, ps=ps, c=c: e.transpose(out=ps[:, 0:34], in_=cwr[0:34, 128 * c:128 * (c + 1)], identity=idf[0:34, 0:34]),
             reads=[cwr_b, idf_b], writes=[psb])
        P.op("dve", lambda e, ps=ps, c=c: e.tensor_copy(out=cw[:, c, :], in_=ps[:, 0:34]), reads=[psb], writes=[cw_b])
    return cw, cw_b


def cum_table(kb, C, gt, gt_b, col0):
    P = kb.P
    tri, tri_b = C["tri_f"]
    one, one_b = C["ones_f"]
    lf = kb.sb("lf", [128, NT, 8], F32)
    lf_b = P.buf("lf")
    P.op("act", lambda e: e.activation(out=lf[:], in_=gt[:, :, col0:col0 + 8], func=AF.Sigmoid), reads=[gt_b], writes=[lf_b])
    P.op("act", lambda e: e.activation(out=lf[:], in_=lf[:], func=AF.Ln), reads=[lf_b], writes=[lf_b])
    lf2 = lf[:].rearrange("p t h -> p (t h)")
    cs, cs_b = kb.bank(6, 8)
    tp, tp_b = kb.bank(6, 8)
    P.op("pe", lambda e: e.matmul(cs[:, 0:256], lhsT=tri[:], rhs=lf2, start=True, stop=True), reads=[lf_b, tri_b], writes=[cs_b])
    P.op("pe", lambda e: e.matmul(tp[:, 0:256], lhsT=one[:], rhs=lf2, start=True, stop=True), reads=[lf_b, one_b], writes=[tp_b])
    tot = kb.sb("tot", [128, NT, 8], F32)
    tot_b = P.buf("tot")
    pin = kb.sb("pin", [128, NT, 8], F32)
    pin_b = P.buf("pin")
    Ft = kb.sb("Ft", [128, NT, 8], F32)
    Ft_b = P.buf("Ft")
    P.op("dve", lambda e: e.tensor_copy(out=tot[:].rearrange("p t h -> p (t h)"), in_=tp[:, 0:256]), reads=[tp_b], writes=[tot_b])
    for h in range(8):
        P.op("dve", lambda e, h=h: e.tensor_tensor_scan(out=pin[:, :, h], data0=one[:, 0:NT], data1=tot[:, :, h], initial=0.0, op0=ALU.mult, op1=ALU.add),
             reads=[tot_b, one_b], writes=[pin_b])
    P.op("dve", lambda e: e.tensor_tensor(out=Ft[:].rearrange("p t h -> p (t h)"), in0=cs[:, 0:256], in1=pin[:].rearrange("p t h -> p (t h)"), op=ALU.add),
         reads=[cs_b, pin_b], writes=[Ft_b])
    P.op("dve", lambda e: e.tensor_tensor(out=Ft[:], in0=Ft[:], in1=tot[:], op=ALU.subtract), reads=[Ft_b, tot_b], writes=[Ft_b])
    return (Ft, Ft_b), (pin, pin_b), (lf, lf_b)


def phase_B0(kb):
    P = kb.P
    sc = float(128 ** -0.5)
    with kb.phase():
        C = load_consts(kb)
        tri_bf, tri_bf_b = C["tri_bf"]
        ones_bf, ones_bf_b = C["ones_bf"]
        cw, cw_b = load_cw(kb, C)
        gt = kb.sb("gt", [128, NT, 16], F32)
        gt_b = P.buf("gt")
        P.dma("sp", lambda e: e.dma_start(out=gt[:], in_=kb.ap["gates"].rearrange("(t p) g -> p t g", p=128)), P.dsem(), reads=kb.db["gates"], writes=[gt_b])
        (Ft, Ft_b), (pin, pin_b), _ = cum_table(kb, C, gt, gt_b, 0)
        q_r = Ring(kb, "q", 2, [128, S], BF16)
        k_r = Ring(kb, "k", 2, [128, S], BF16)
        v_r = Ring(kb, "v", 2, [128, NT, 128], BF16)
        gp_r = Ring(kb, "gp", 2, [128, 32 + S], BF16)
        dgw_r = Ring(kb, "dgw", 2, [128, 31, 128], BF16)
        ident_bf, ident_bf_b = C["ident_bf"]
        bias_r = Ring(kb, "bias", 2, [128, NB, NT], F32)
        pT_r = Ring(kb, "pT", 6, [128, 512], BF16)
        rc_r = Ring(kb, "rc", 2, [128, 512], F32)
        os_r = Ring(kb, "os", 3, [128, 512], BF16)
        acc_r = Ring(kb, "acc", 3, [128, 512], F32)
        for i in range(2):
            P.op("pool", lambda e, i=i: e.memset(gp_r.t[i][:, 0:32], 0.0), writes=[gp_r.b[i]])

        def load_head(h):
            qT, q_b, ql, _ = q_r.next()
            kT, k_b, kl, _ = k_r.next()
            v, v_b, vl, _ = v_r.next()
            gp, gp_b, gl, _ = gp_r.next()
            P.dma("sp", lambda e: e.dma_start(out=kT[:], in_=kb.ap["kT"][h]), kl, reads=kb.db["kT"], writes=[k_b])
            P.dma("sp", lambda e: e.dma_start(out=qT[:], in_=kb.ap["qT"][h]), ql, reads=kb.db["qT"], writes=[q_b])
            P.dma("sp", lambda e: e.dma_start(out=v[:], in_=kb.ap["v"][:, 128 * h:128 * (h + 1)].rearrange("(t p) d -> p t d", p=128)), vl, reads=kb.db["v"], writes=[v_b])
            P.dma("sp", lambda e: e.dma_start(out=gp[:, 32:32 + S], in_=kb.ap["g"][h]), gl, reads=kb.db["g"], writes=[gp_b])
            return (qT, q_b, kT, k_b, v, v_b, gp, gp_b)

        def build_dgw(hh):
            dgw, dgw_b, _, _ = dgw_r.next()
            for tap in range(31):
                P.op("pool", lambda e: e.tensor_scalar(out=dgw[:, tap, :], in0=ident_bf[:], scalar1=cw[:, hh, tap:tap + 1], scalar2=0.0, op0=ALU.mult, op1=ALU.add),
                     reads=[ident_bf_b, cw_b], writes=[dgw_b])
            return dgw, dgw_b

        nxt_head = load_head(0)
        s_i = [0]
        conv_q = []
        for h in range(8):
            qT, q_b, kT, k_b, v, v_b, gp, gp_b = nxt_head
            if h + 1 < 8:
                nxt_head = load_head(h + 1)
            bias, bias_b, _, _ = bias_r.next()
            if h == 0:
                nxt_dgw = build_dgw(0)
            dgw, dgw_b = nxt_dgw
            if h + 1 < 8:
                nxt_dgw = build_dgw(h + 1)
            for j in range(NB):
                P.op("dve", lambda e, j=j, bias=bias: e.tensor_scalar(out=bias[:, j, :], in0=Ft[:, :, h], scalar1=-1.0, scalar2=pin[:, 4 * j + 1, h:h + 1], op0=ALU.mult, op1=ALU.add),
                     reads=[Ft_b, pin_b], writes=[bias_b])
            for j in range(NB):
                nt = 4 * j + 4
                left = (8 - h) * NB - j
                pump(kb, n=(len(kb.cq) + left - 1) // left, gate=[k_b])
                O, O_b = kb.ps[3 + (j % 2)], kb.pb[3 + (j % 2)]
                Dn, Dn_b = kb.ps[5 + (j % 2)], kb.pb[5 + (j % 2)]

                def emitS(t):
                    s_i[0] = (s_i[0] + 1) % 3
                    Sps, S_b = kb.ps[s_i[0]], kb.pb[s_i[0]]
                    c0 = max(0, 128 * (t - 4 * j))
                    P.op("pe", lambda e: e.matmul(Sps[:, c0:512], lhsT=kT[:, 128 * t:128 * (t + 1)], rhs=qT[:, 512 * j + c0:512 * (j + 1)], start=True, stop=True),
                         reads=[k_b, q_b], writes=[S_b])
                    return Sps, S_b, c0

                sq = [emitS(0)]
                if nt > 1:
                    sq.append(emitS(1))
                for t in range(nt):
                    if t + 2 < nt:
                        sq.append(emitS(t + 2))
                    Sps, S_b, c0 = sq.pop(0)
                    pT, pT_b, _, _ = pT_r.next()
                    P.op("act", lambda e, Sps=Sps, c0=c0, pT=pT, t=t: e.activation(out=pT[:, c0:512], in_=Sps[:, c0:512], func=AF.Exp, bias=bias[:, j, t:t + 1], scale=sc),
                         reads=[S_b, bias_b], writes=[pT_b])
                    if t >= 4 * j:
                        P.op("dve", lambda e, c0=c0, pT=pT: e.tensor_tensor(out=pT[:, c0:c0 + 128], in0=pT[:, c0:c0 + 128], in1=tri_bf[:], op=ALU.mult),
                             reads=[pT_b, tri_bf_b], writes=[pT_b])
                    P.op("pe", lambda e, c0=c0, pT=pT, t=t: e.matmul(O[:, c0:512], lhsT=v[:, t, :], rhs=pT[:, c0:512], start=(t == 0), stop=(t == nt - 1)),
                         reads=[v_b, pT_b], writes=[O_b], pe_accum=True)
                    P.op("pe", lambda e, c0=c0, pT=pT, t=t: e.matmul(Dn[:, c0:512], lhsT=ones_bf[:], rhs=pT[:, c0:512], start=(t == 0), stop=(t == nt - 1)),
                         reads=[ones_bf_b, pT_b], writes=[Dn_b], pe_accum=True)
                    for _ in range((31 + nt - 1) // nt):
                        if conv_q:
                            conv_q.pop(0)()
                rc, rc_b, _, _ = rc_r.next()
                os_, os_b, _, ost = os_r.next()
                P.op("dve", lambda e, rc=rc: e.reciprocal(out=rc[:], in_=Dn[:]), reads=[Dn_b], writes=[rc_b])
                P.op("dve", lambda e, rc=rc, os_=os_: e.tensor_tensor(out=os_[:], in0=O[:], in1=rc[:], op=ALU.mult), reads=[O_b, rc_b], writes=[os_b])
                P.dma("sp", lambda e, os_=os_: e.dma_start(out=kb.ap["mixT"][h, :, 512 * j:512 * (j + 1)], in_=os_[:]), ost, reads=[os_b], writes=[kb.db["mixT"][j]])
                while conv_q:
                    conv_q.pop(0)()
                acc, acc_b, _, ast = acc_r.next()
                base = 2 + 512 * j
                cps, cps_b = kb.ps[7], kb.pb[7]

                def mk(tap, cps=cps, cps_b=cps_b, base=base, dgw=dgw, dgw_b=dgw_b, gp=gp, gp_b=gp_b):
                    return lambda: P.op("pe", lambda e: e.matmul(cps[:], lhsT=dgw[:, tap, :], rhs=gp[:, base + tap:base + tap + 512], start=(tap == 0), stop=(tap == 30)),
                                        reads=[dgw_b, gp_b], writes=[cps_b], pe_accum=True)

                def fin(cps=cps, cps_b=cps_b, acc=acc, acc_b=acc_b, ast=ast, h=h, j=j):
                    P.op("act", lambda e: e.activation(out=acc[:], in_=cps[:], func=AF.Identity, bias=cw[:, h, 31:32], scale=1.0), reads=[cps_b, cw_b], writes=[acc_b])
                    P.dma("sp", lambda e: e.dma_start(out=kb.ap["co"][h, :, 512 * j:512 * (j + 1)], in_=acc[:]), ast, reads=[acc_b], writes=[kb.db["co"][j]])

                conv_q.extend([mk(tap) for tap in range(31)] + [fin])
            while conv_q:
                conv_q.pop(0)()


def phase_B2(kb):
    P = kb.P
    with kb.phase():
        C = load_consts(kb)
        one, one_b = C["ones_f"]
        cw, cw_b = load_cw(kb, C)
        co_r = Ring(kb, "co", 2, [128, 8, 512], F32)
        sq_r = Ring(kb, "sq", 2, [128, 8, 512], F32)
        st_r = Ring(kb, "st", 2, [128, 3, 512], F32)
        out_r = Ring(kb, "cvo", 2, [128, 8, 512], BF16)
        for tb in range(NB):
            co, co_b, col, _ = co_r.next()
            P.dma("sp", lambda e, co=co: e.dma_start(out=co[:], in_=kb.ap["co"][:, :, 512 * tb:512 * (tb + 1)].rearrange("c p t -> p c t")), col, reads=[kb.db["co"][tb]], writes=[co_b])
            sq, sq_b, _, _ = sq_r.next()
            P.op("act", lambda e, co=co, sq=sq: e.activation(out=sq[:], in_=co[:], func=AF.Square), reads=[co_b], writes=[sq_b])
            S1, S1_b = kb.bank(0, 6)
            S2, S2_b = kb.bank(0, 6)
            for c in range(8):
                P.op("pe", lambda e, c=c, co=co, S1=S1: e.matmul(S1[:], lhsT=one[:], rhs=co[:, c, :], start=(c == 0), stop=(c == 7)), reads=[co_b, one_b], writes=[S1_b], pe_accum=True)
            for c in range(8):
                P.op("pe", lambda e, c=c, sq=sq, S2=S2: e.matmul(S2[:], lhsT=one[:], rhs=sq[:, c, :], start=(c == 0), stop=(c == 7)), reads=[sq_b, one_b], writes=[S2_b], pe_accum=True)
            stt, st_b, _, _ = st_r.next()
            mean, msq, rstd = stt[:, 0, :], stt[:, 1, :], stt[:, 2, :]
            P.op("act", lambda e, mean=mean, S1=S1: e.mul(out=mean, in_=S1[:], mul=1.0 / 1024), reads=[S1_b], writes=[st_b])
            P.op("dve", lambda e, mean=mean, msq=msq: e.tensor_tensor(out=msq, in0=mean, in1=mean, op=ALU.mult), reads=[st_b], writes=[st_b])
            P.op("dve", lambda e, msq=msq, rstd=rstd, S2=S2: e.scalar_tensor_tensor(out=rstd, in0=S2[:], scalar=1.0 / 1024, in1=msq, op0=ALU.mult, op1=ALU.subtract), reads=[S2_b, st_b], writes=[st_b])
            P.op("dve", lambda e, rstd=rstd: e.tensor_scalar_add(out=rstd, in0=rstd, scalar1=EPS), reads=[st_b], writes=[st_b])
            P.op("act", lambda e, rstd=rstd: e.sqrt(out=rstd, in_=rstd), reads=[st_b], writes=[st_b])
            P.op("dve", lambda e, rstd=rstd: e.reciprocal(out=rstd, in_=rstd), reads=[st_b], writes=[st_b])
            o, o_b, _, ost = out_r.next()
            for c in range(8):
                P.op("dve", lambda e, c=c, co=co, mean=mean: e.tensor_tensor(out=co[:, c, :], in0=co[:, c, :], in1=mean, op=ALU.subtract), reads=[co_b, st_b], writes=[co_b])
                P.op("pool", lambda e, c=c, co=co, rstd=rstd: e.tensor_tensor(out=co[:, c, :], in0=co[:, c, :], in1=rstd, op=ALU.mult), reads=[co_b, st_b], writes=[co_b])
                P.op("act", lambda e, c=c, co=co, o=o: e.activation(out=o[:, c, :], in_=co[:, c, :], func=AF.Silu, scale=cw[:, c, 32:33], bias=cw[:, c, 33:34]), reads=[co_b, cw_b], writes=[o_b])
            P.dma("act", lambda e, o=o: e.dma_start(out=kb.ap["mixT"][8:16, :, 512 * tb:512 * (tb + 1)].rearrange("c p t -> p c t"), in_=o[:]), ost, reads=[o_b], writes=[kb.db["mixT"][tb]])


def phase_B1(kb):
    P = kb.P
    with kb.phase():
        C = load_consts(kb)
        ident, ident_b = C["ident_bf"]
        tri_bf, tri_bf_b = C["tri_bf"]
        ones_bf, ones_bf_b = C["ones_bf"]
        ones_f, ones_f_b = C["ones_f"]
        ident_f, ident_f_b = C["ident_f"]
        gt = kb.sb("gt", [128, NT, 16], F32)
        gt_b = P.buf("gt")
        P.dma("sp", lambda e: e.dma_start(out=gt[:], in_=kb.ap["gates"].rearrange("(t p) g -> p t g", p=128)), P.dsem(), reads=kb.db["gates"], writes=[gt_b])
        (Ft, Ft_b), (pin, pin_b), _ = cum_table(kb, C, gt, gt_b, 8)
        brt = kb.sb("brt", [128, NT, 8], F32)
        brt_b = P.buf("brt")
        aa = kb.sb("aa", [128, NT, 8], F32)
        aa_b = P.buf("aa")
        ee = kb.sb("ee", [128, NB, 8], F32)
        ee_b = P.buf("ee")
        P.op("dve", lambda e: e.memset(brt[:, 0:4, :], 0.0), writes=[brt_b])
        for j in range(1, NB):
            for tl in range(4):
                P.op("dve", lambda e: e.tensor_copy(out=brt[:, 4 * j + tl, :], in_=pin[:, 4 * j - 1, :]), reads=[pin_b], writes=[brt_b])
        P.op("dve", lambda e: e.tensor_tensor(out=aa[:], in0=gt[:, :, 0:8], in1=Ft[:], op=ALU.subtract), reads=[gt_b, Ft_b], writes=[aa_b])
        P.op("dve", lambda e: e.tensor_tensor(out=aa[:], in0=aa[:], in1=brt[:], op=ALU.add), reads=[aa_b, brt_b], writes=[aa_b])
        P.op("act", lambda e: e.activation(out=aa[:], in_=aa[:], func=AF.Exp), reads=[aa_b], writes=[aa_b])
        for j in range(NB):
            P.op("dve", lambda e: e.tensor_tensor(out=ee[:, j, :], in0=pin[:, 4 * j + 3, :], in1=brt[:, 4 * j, :], op=ALU.subtract), reads=[pin_b, brt_b], writes=[ee_b])
        P.op("act", lambda e: e.activation(out=ee[:], in_=ee[:], func=AF.Exp), reads=[ee_b], writes=[ee_b])

        Cst = kb.sb("Cst", [128, 8, 260], F32)
        Cbf = kb.sb("Cbf", [128, 8, 256], BF16)
        nbc = kb.sb("nbc", [128, 8, 128], BF16)
        cst_b = [P.buf() for _ in range(8)]
        cbf_b = [P.buf() for _ in range(8)]
        P.op("pool", lambda e: e.memset(Cst[:], 0.0), writes=cst_b)

        q_r = Ring(kb, "qb", 2, [128, 8, 512], BF16)
        k_r = Ring(kb, "kb", 2, [128, 8, 512], BF16)
        v_r = Ring(kb, "vb", 2, [128, 4, D], BF16)
        o_r = Ring(kb, "ob", 2, [128, 16, 512], BF16)
        ktm_r = Ring(kb, "ktm", 3, [128, 4, 128], BF16)
        vp_r = Ring(kb, "vp", 3, [128, 4, 264], BF16)
        abc_r = Ring(kb, "abc", 2, [128, 8, 4, 128], BF16)
        dg_r = Ring(kb, "dg", 2, [128, 4, 128], F32)
        ei_r = Ring(kb, "ei", 3, [128, 512], F32)
        pT_r = Ring(kb, "pT", 4, [128, 512], BF16)
        dm_r = Ring(kb, "dm", 2, [128, 512], F32)
        t1_r = Ring(kb, "t1", 3, [128, 512], F32)
        ms_r = Ring(kb, "ms", 2, [128, 2, 512], BF16)

        def load_blk(j):
            qb, qb_b, ql, _ = q_r.next()
            kk, kk_b, kl, _ = k_r.next()
            vb, vb_b, vl, _ = v_r.next()
            ob, ob_b, ol, _ = o_r.next()
            sl = slice(512 * j, 512 * (j + 1))
            P.dma("sp", lambda e: e.dma_start(out=kk[:], in_=kb.ap["kT"][:, :, sl].rearrange("h p t -> p h t")), kl, reads=[kb.db["kT"][j]], writes=[kk_b])
            P.dma("sp", lambda e: e.dma_start(out=qb[:], in_=kb.ap["qT"][:, :, sl].rearrange("h p t -> p h t")), ql, reads=[kb.db["qT"][j]], writes=[qb_b])
            P.dma("sp", lambda e: e.dma_start(out=vb[:], in_=kb.ap["v"][sl, :].rearrange("(t p) d -> p t d", p=128)), vl, reads=[kb.db["v"][j]], writes=[vb_b])
            P.dma("sp", lambda e: e.dma_start(out=ob[:], in_=kb.ap["oT"][:, :, sl].rearrange("c p t -> p c t")), ol, reads=[kb.db["oT"][j]], writes=[ob_b])
            return dict(qb=qb, qb_b=qb_b, kk=kk, kk_b=kk_b, vb=vb, vb_b=vb_b, ob=ob, ob_b=ob_b)

        def prep_head(L, j, h):
            c = {}
            kk, kk_b, vb, vb_b = L["kk"], L["kk_b"], L["vb"], L["vb_b"]
            ktm, ktm_b, _, _ = ktm_r.next()
            if j + 1 < NB:
                kt, kt_b = kb.ps[5], kb.pb[5]
                ktv = kt.bitcast(BF16)
                for tl in range(4):
                    P.op("pe", lambda e: e.transpose(out=ktv[:, 128 * tl:128 * (tl + 1)], in_=kk[:, h, 128 * tl:128 * (tl + 1)], identity=ident[:]),
                         reads=[kk_b, ident_b], writes=[kt_b], pe_accum=True)
                P.op("act", lambda e: e.copy(out=ktm[:].rearrange("p a c -> p (a c)"), in_=ktv[:, 0:512]), reads=[kt_b], writes=[ktm_b])
            vp, vp_b, _, _ = vp_r.next()
            for tl in range(4):
                P.op("dve", lambda e: e.tensor_scalar_mul(out=vp[:, tl, 0:256], in0=vb[:, tl, 256 * h:256 * (h + 1)], scalar1=aa[:, 4 * j + tl, h:h + 1]),
                     reads=[vb_b, aa_b], writes=[vp_b])
            P.op("act", lambda e: e.copy(out=vp[:, :, 256], in_=aa[:, 4 * j:4 * j + 4, h]), reads=[aa_b], writes=[vp_b])
            dg, dg_b, _, _ = dg_r.next()
            P.op("pool", lambda e: e.tensor_tensor(out=dg[:], in0=ident_f[:].unsqueeze(1).to_broadcast([128, 4, 128]), in1=Ft[:, 4 * j:4 * j + 4, h:h + 1].to_broadcast([128, 4, 128]), op=ALU.mult),
                 reads=[ident_f_b, Ft_b], writes=[dg_b])
            Bbc, Bbc_b = kb.ps[7], kb.pb[7]
            P.op("pe", lambda e: e.matmul(Bbc[:], lhsT=ones_f[:], rhs=dg[:].rearrange("p a c -> p (a c)"), start=True, stop=True), reads=[ones_f_b, dg_b], writes=[Bbc_b])
            ei, ei_b, _, _ = ei_r.next()
            P.op("act", lambda e: e.activation(out=ei[:], in_=Bbc[:], func=AF.Exp, scale=-1.0, bias=brt[:, 4 * j, h:h + 1]), reads=[Bbc_b, brt_b], writes=[ei_b])
            c.update(ktm=ktm, ktm_b=ktm_b, vp=vp, vp_b=vp_b, ei=ei, ei_b=ei_b)
            return c

        def main_head(L, c, abc, abc_b, j, h):
            qb, qb_b, kk, kk_b, ob, ob_b = L["qb"], L["qb_b"], L["kk"], L["kk_b"], L["ob"], L["ob_b"]
            vp, vp_b, ei, ei_b = c["vp"], c["vp_b"], c["ei"], c["ei_b"]
            O0, O0_b = kb.ps[2], kb.pb[2]
            O1, O1_b = kb.ps[3], kb.pb[3]
            Dn, Dn_b = kb.ps[4], kb.pb[4]
            first = True
            if j > 0:
                P.op("pe", lambda e: e.matmul(O0[:], lhsT=Cbf[:, h, 0:128], rhs=qb[:, h, :], start=True, stop=False), reads=[cbf_b[h], qb_b], writes=[O0_b], pe_accum=True)
                P.op("pe", lambda e: e.matmul(O1[:], lhsT=Cbf[:, h, 128:256], rhs=qb[:, h, :], start=True, stop=False), reads=[cbf_b[h], qb_b], writes=[O1_b], pe_accum=True)
                P.op("pe", lambda e: e.matmul(Dn[:], lhsT=nbc[:, h, :], rhs=qb[:, h, :], start=True, stop=False), reads=[cbf_b[h], qb_b], writes=[Dn_b], pe_accum=True)
                first = False
            for tl in range(4):
                c0 = 128 * tl
                Sps, S_b = kb.bank(0, 2)
                P.op("pe", lambda e: e.matmul(Sps[:, c0:512], lhsT=kk[:, h, 128 * tl:128 * (tl + 1)], rhs=qb[:, h, c0:512], start=True, stop=True), reads=[kk_b, qb_b], writes=[S_b])
                pT, pT_b, _, _ = pT_r.next()
                if c0 + 128 < 512:
                    P.op("act", lambda e: e.copy(out=pT[:, c0 + 128:512], in_=Sps[:, c0 + 128:512]), reads=[S_b], writes=[pT_b])
                P.op("dve", lambda e: e.tensor_tensor(out=pT[:, c0:c0 + 128], in0=Sps[:, c0:c0 + 128], in1=tri_bf[:], op=ALU.mult), reads=[S_b, tri_bf_b], writes=[pT_b])
                st_, sp_ = first, (tl == 3)
                P.op("pe", lambda e: e.matmul(O0[:, c0:512], lhsT=vp[:, tl, 0:128], rhs=pT[:, c0:512], start=st_, stop=sp_), reads=[vp_b, pT_b], writes=[O0_b], pe_accum=True)
                P.op("pe", lambda e: e.matmul(O1[:, c0:512], lhsT=vp[:, tl, 128:256], rhs=pT[:, c0:512], start=st_, stop=sp_), reads=[vp_b, pT_b], writes=[O1_b], pe_accum=True)
                P.op("pe", lambda e: e.matmul(Dn[:, c0:512], lhsT=abc[:, h, tl, :], rhs=pT[:, c0:512], start=st_, stop=sp_), reads=[abc_b, pT_b], writes=[Dn_b], pe_accum=True)
                first = False
            dm, dm_b, _, _ = dm_r.next()
            P.op("act", lambda e: e.activation(out=dm[:], in_=Dn[:], func=AF.Abs), reads=[Dn_b], writes=[dm_b])
            P.op("dve", lambda e: e.tensor_tensor(out=dm[:], in0=dm[:], in1=ei[:], op=ALU.max), reads=[dm_b, ei_b], writes=[dm_b])
            P.op("dve", lambda e: e.reciprocal(out=dm[:], in_=dm[:]), reads=[dm_b], writes=[dm_b])
            ms, ms_b, _, mst = ms_r.next()
            for half, (Oh, Oh_b) in enumerate(((O0, O0_b), (O1, O1_b))):
                t1, t1_b, _, _ = t1_r.next()
                P.op("dve", lambda e: e.tensor_tensor(out=t1[:], in0=Oh[:], in1=dm[:], op=ALU.mult), reads=[Oh_b, dm_b], writes=[t1_b])
                P.op("pool", lambda e: e.tensor_tensor(out=ms[:, half, :], in0=t1[:], in1=ob[:, 2 * h + half, :], op=ALU.mult), reads=[t1_b, ob_b], writes=[ms_b])
            P.dma("pool", lambda e: e.dma_start(out=kb.ap["mixT"][2 * h:2 * h + 2, :, 512 * j:512 * (j + 1)].rearrange("c p t -> p c t"), in_=ms[:]), mst, reads=[ms_b], writes=[kb.db["mixT"][j]])

        def state_head(c, j, h):
            if j + 1 >= NB:
                return
            ktm, ktm_b, vp, vp_b = c["ktm"], c["ktm_b"], c["vp"], c["vp_b"]
            Cps, Cps_b = kb.ps[6], kb.pb[6]
            for tl in range(4):
                P.op("pe", lambda e: e.matmul(Cps[:, 0:257], lhsT=ktm[:, tl, :], rhs=vp[:, tl, 0:257], start=(tl == 0), stop=(tl == 3)), reads=[ktm_b, vp_b], writes=[Cps_b], pe_accum=True)
            P.op("dve", lambda e: e.tensor_scalar_mul(out=Cst[:, h, 0:257], in0=Cst[:, h, 0:257], scalar1=ee[:, j, h:h + 1]), reads=[cst_b[h], ee_b], writes=[cst_b[h]])
            P.op("dve", lambda e: e.scalar_tensor_tensor(out=Cst[:, h, 0:257], in0=Cps[:, 0:257], scalar=ee[:, j, h:h + 1], in1=Cst[:, h, 0:257], op0=ALU.mult, op1=ALU.add),
                 reads=[Cps_b, cst_b[h], ee_b], writes=[cst_b[h]])
            P.op("act", lambda e: e.copy(out=Cbf[:, h, :], in_=Cst[:, h, 0:256]), reads=[cst_b[h]], writes=[cbf_b[h]])
            P.op("pool", lambda e: e.tensor_scalar(out=nbc[:, h, :], in0=ones_bf[:], scalar1=Cst[:, h, 256:257], scalar2=0.0, op0=ALU.mult, op1=ALU.add), reads=[ones_bf_b, cst_b[h]], writes=[cbf_b[h]])

        nxt = load_blk(0)
        for j in range(NB):
            L = nxt
            if j + 1 < NB:
                nxt = load_blk(j + 1)
            abc, abc_b, _, _ = abc_r.next()
            P.op("pool", lambda e: e.tensor_copy(out=abc[:], in_=aa[:, 4 * j:4 * j + 4, :].rearrange("p t h -> p h t").unsqueeze(3).to_broadcast([128, 8, 4, 128])),
                 reads=[aa_b], writes=[abc_b])
            cs = {0: prep_head(L, j, 0)}
            for h in range(8):
                if h + 1 < 8:
                    cs[h + 1] = prep_head(L, j, h + 1)
                main_head(L, cs[h], abc, abc_b, j, h)
                if h > 0:
                    state_head(cs[h - 1], j, h - 1)
            state_head(cs[7], j, 7)


def load_bcast(kb, name, row):
    P = kb.P
    t = kb.sb("bc_" + name, [128, D], F32)
    b = P.buf()
    P.dma("sp", lambda e: e.dma_start(out=t[:], in_=kb.ap[name][row:row + 1, :].partition_broadcast(128)), P.dsem(), writes=[b])
    return t, b


def ln_tile(kb, r, r_b, g, g_b, bb, bb_b, sm_r):
    P = kb.P
    sm, sm_b, _, _ = sm_r.next()
    for c in range(4):
        P.op("dve", lambda e, c=c: e.bn_stats(out=sm[:, 6 * c:6 * c + 6], in_=r[:, 512 * c:512 * (c + 1)]), reads=r_b, writes=[sm_b])
    P.op("dve", lambda e: e.bn_aggr(out=sm[:, 24:26], in_=sm[:, 0:24]), reads=[sm_b], writes=[sm_b])
    P.op("dve", lambda e: e.tensor_scalar_add(out=sm[:, 26:27], in0=sm[:, 25:26], scalar1=EPS), reads=[sm_b], writes=[sm_b])
    P.op("act", lambda e: e.sqrt(out=sm[:, 26:27], in_=sm[:, 26:27]), reads=[sm_b], writes=[sm_b])
    P.op("dve", lambda e: e.reciprocal(out=sm[:, 26:27], in_=sm[:, 26:27]), reads=[sm_b], writes=[sm_b])
    P.op("dve", lambda e: e.scalar_tensor_tensor(out=sm[:, 27:28], in0=sm[:, 24:25], scalar=-1.0, in1=sm[:, 26:27], op0=ALU.mult, op1=ALU.mult), reads=[sm_b], writes=[sm_b])
    P.op("act", lambda e: e.activation(out=r, in_=r, func=AF.Identity, scale=sm[:, 26:27], bias=sm[:, 27:28]), reads=r_b + [sm_b], writes=r_b)
    H = D // 2
    lo_b, hi_b = r_b[:len(r_b) // 2], r_b[len(r_b) // 2:]
    P.op("pool", lambda e: e.tensor_tensor(out=r[:, 0:H], in0=r[:, 0:H], in1=g[:, 0:H], op=ALU.mult), reads=lo_b + [g_b], writes=lo_b)
    P.op("dve", lambda e: e.tensor_tensor(out=r[:, H:D], in0=r[:, H:D], in1=g[:, H:D], op=ALU.mult), reads=hi_b + [g_b], writes=hi_b)
    P.op("dve", lambda e: e.tensor_tensor(out=r[:, 0:H], in0=r[:, 0:H], in1=bb[:, 0:H], op=ALU.add), reads=lo_b + [bb_b], writes=lo_b)
    P.op("pool", lambda e: e.tensor_tensor(out=r[:, H:D], in0=r[:, H:D], in1=bb[:, H:D], op=ALU.add), reads=hi_b + [bb_b], writes=hi_b)


def phase_C(kb, layer, xres_name, wname, ln_g, ln_b, out_name, outT_name):
    P = kb.P
    wv = kb.ap[wname].rearrange("(kc p) n -> p kc n", p=128)
    wbufs = need_w(kb, wname)
    with kb.phase():
        C = load_consts(kb)
        ident, ident_b = C["ident_bf"]
        m_r = Ring(kb, "mT", 2, [128, 16, 512], BF16)
        xr_r = Ring(kb, "xr", 2, [128, 4, D], F32)
        xr_tb = [[[P.buf() for _ in range(4)] for _ in range(4)] for _ in range(2)]
        wres = kb.sb("wres", [128, 16, D], BF16)
        wres_b = [P.buf() for _ in range(4)]
        wds = [P.dsem() for _ in range(4)]
        P.dma("sp", lambda e: e.dma_start(out=wres[:, :, 0:512], in_=wv[:, :, 0:512]), wds[0], reads=wbufs, writes=[wres_b[0]])
        mT0, mT0_b, ml0, _ = m_r.next()
        m_r.i -= 1
        P.dma("sp", lambda e: e.dma_start(out=mT0[:], in_=kb.ap["mixT"][:, :, 0:512].rearrange("c p t -> p c t")), ml0, reads=[kb.db["mixT"][0]], writes=[mT0_b])
        first_m = dict(tb=0, mT=mT0, mT_b=mT0_b)
        for n in range(1, 4):
            P.dma("sp", lambda e: e.dma_start(out=wres[:, :, 512 * n:512 * (n + 1)], in_=wv[:, :, 512 * n:512 * (n + 1)]), wds[n], reads=wbufs, writes=[wres_b[n]])
        g, g_b = load_bcast(kb, ln_g, layer)
        bb, bb_b = load_bcast(kb, ln_b, layer)
        xb_r = Ring(kb, "xb", 2, [128, D], BF16)
        oT_r = Ring(kb, "oT", 1, [128, 16, 512], BF16)
        sm_r = Ring(kb, "sm", 4, [128, 32], F32)

        def load_m(tb):
            mT, mT_b, ml, _ = m_r.next()
            P.dma("sp", lambda e: e.dma_start(out=mT[:], in_=kb.ap["mixT"][:, :, 512 * tb:512 * (tb + 1)].rearrange("c p t -> p c t")), ml, reads=[kb.db["mixT"][tb]], writes=[mT_b])
            return dict(tb=tb, mT=mT, mT_b=mT_b)

        def load_x(c):
            tb = c["tb"]
            xr, _, xl, _ = xr_r.next()
            sl = xr_r.i % 2
            tbs = xr_tb[sl]
            for n in range(4):
                P.dma("pool", lambda e: e.dma_start(out=xr[:, :, 512 * n:512 * (n + 1)], in_=kb.ap[xres_name][512 * tb:512 * (tb + 1), 512 * n:512 * (n + 1)].rearrange("(t p) d -> p t d", p=128)),
                      xl, reads=[kb.db[xres_name][tb]], writes=[tbs[t][n] for t in range(4)])
            c.update(xr=xr, tbs=tbs, st=xr_r.st[sl])
            return c

        def epi_ln(c, t):
            r = c["xr"][:, t, :]
            ln_tile(kb, r, c["tbs"][t], g, g_b, bb, bb_b, sm_r)
            xb, xb_b = xb_r.next()[:2]
            ce = "act" if kb.alt() else "dve"
            if ce == "act":
                P.op("act", lambda e: e.copy(out=xb[:], in_=r), reads=c["tbs"][t], writes=[xb_b])
            else:
                P.op("dve", lambda e: e.tensor_copy(out=xb[:], in_=r), reads=c["tbs"][t], writes=[xb_b])
            c["xb%d" % t] = (xb, xb_b)
            tb = c["tb"]
            r0 = 512 * tb + 128 * t
            P.dma("act", lambda e: e.dma_start(out=kb.ap[out_name][r0:r0 + 128, :], in_=r), c["st"], reads=c["tbs"][t], writes=[kb.db[out_name][tb]])

        def epi_tr(c, t):
            if t == 0:
                c["oT"] = oT_r.next()
            oT, oT_b, _, ost = c["oT"]
            xb, xb_b = c["xb%d" % t]
            transpose_tile(kb, xb, xb_b, D, oT, oT_b, 128 * t, ident[:], ident_b)
            if t == 3:
                tb = c["tb"]
                P.dma("act", lambda e: e.dma_start(out=kb.ap[outT_name][:, :, 512 * tb:512 * (tb + 1)].rearrange("c p t -> p c t"), in_=oT[:]), ost, reads=[oT_b], writes=[kb.db[outT_name][tb]])

        m_r.i += 1
        nxt = load_x(first_m)
        pend = None
        for tb in range(NB):
            c = nxt
            mT, mT_b, xr, tbs = c["mT"], c["mT_b"], c["xr"], c["tbs"]
            pump(kb, n=4, gate=[mT_b])
            for n in range(4):
                wt, wb_ = wres[:, :, 512 * n:512 * (n + 1)], wres_b[n]
                if n == 1 and tb + 1 < NB:
                    nxt = load_m(tb + 1)
                if pend is not None and n in (0, 2):
                    epi_ln(pend, n)
                    epi_ln(pend, n + 1)
                for t in range(4):
                    ps, psb = kb.bank(2, 8)
                    for kc in range(16):
                        P.op("pe", lambda e, kc=kc: e.matmul(ps[:], lhsT=mT[:, kc, 128 * t:128 * (t + 1)], rhs=wt[:, kc, :], start=(kc == 0), stop=(kc == 15)),
                             reads=[mT_b, wb_], writes=[psb], pe_accum=True)
                    sl = xr[:, t, 512 * n:512 * (n + 1)]
                    P.op("dve", lambda e: e.scalar_tensor_tensor(out=sl, in0=sl, scalar=ALPHA, in1=ps[:], op0=ALU.mult, op1=ALU.add), reads=[psb, tbs[t][n]], writes=[tbs[t][n]])
                if pend is not None and n in (1, 3):
                    epi_tr(pend, n - 1)
                    epi_tr(pend, n)
                if n == 3 and tb + 1 < NB:
                    nxt = load_x(nxt)
            pend = c
        for t in range(4):
            epi_ln(pend, t)
            epi_tr(pend, t)


def phase_D(kb, layer, in_name, inT_name, out_name, outT_name):
    P = kb.P
    wu = kb.ap[f"wb_up{layer}"].rearrange("(kc p) n -> p kc n", p=128)
    wd = kb.ap[f"wb_down{layer}"].rearrange("(kc p) n -> p kc n", p=128)
    wu_b, wd_b = need_w(kb, f"wb_up{layer}"), need_w(kb, f"wb_down{layer}")
    with kb.phase():
        C = load_consts(kb)
        ident, ident_b = C["ident_bf"]
        g, g_b = load_bcast(kb, "ln_ffn_g", layer)
        bb, bb_b = load_bcast(kb, "ln_ffn_b", layer)
        xT_r = Ring(kb, "xT", 1, [128, 16, 512], BF16)
        hT = kb.sb("hT", [128, 64, 512], BF16)
        hT_b = [P.buf() for _ in range(64)]
        w_r = Ring(kb, "wst", 3, [128, 16, 512], BF16)
        xr_r = Ring(kb, "xr", 1, [128, 4, D], F32)
        tbs = [[P.buf() for _ in range(4)] for _ in range(4)]
        xb_r = Ring(kb, "xb", 2, [128, D], BF16)
        oT_r = Ring(kb, "oT", 1, [128, 16, 512], BF16)
        sm_r = Ring(kb, "sm", 4, [128, 32], F32)
        tmp_r = Ring(kb, "tmp", 2, [128, 512], F32)

        def load_xT(tb):
            xT, xT_b, xl, _ = xT_r.next()
            P.dma("sp", lambda e: e.dma_start(out=xT[:], in_=kb.ap[inT_name][:, :, 512 * tb:512 * (tb + 1)].rearrange("c p t -> p c t")), xl, reads=[kb.db[inT_name][tb]], writes=[xT_b])
            return xT, xT_b

        def load_x(tb):
            xr, _, xrl, xrs = xr_r.next()
            for n in range(4):
                P.dma("pool", lambda e: e.dma_start(out=xr[:, :, 512 * n:512 * (n + 1)], in_=kb.ap[in_name][512 * tb:512 * (tb + 1), 512 * n:512 * (n + 1)].rearrange("(t p) d -> p t d", p=128)),
                      xrl, reads=[kb.db[in_name][tb]], writes=[tbs[t][n] for t in range(4)])
            return xr, xrs

        def epi_ln(c, t):
            r = c["xr"][:, t, :]
            ln_tile(kb, r, tbs[t], g, g_b, bb, bb_b, sm_r)
            xb, xb_b = xb_r.next()[:2]
            if kb.alt():
                P.op("act", lambda e: e.copy(out=xb[:], in_=r), reads=tbs[t], writes=[xb_b])
            else:
                P.op("dve", lambda e: e.tensor_copy(out=xb[:], in_=r), reads=tbs[t], writes=[xb_b])
            c["xb%d" % t] = (xb, xb_b)
            tb = c["tb"]
            r0 = 512 * tb + 128 * t
            P.dma("act", lambda e: e.dma_start(out=kb.ap[out_name][r0:r0 + 128, :], in_=r), c["xrs"], reads=tbs[t], writes=[kb.db[out_name][tb]])

        def epi_tr(c, t):
            if t == 0:
                c["oT"] = oT_r.next()
            oT, oT_b, _, ost = c["oT"]
            xb, xb_b = c["xb%d" % t]
            transpose_tile(kb, xb, xb_b, D, oT, oT_b, 128 * t, ident[:], ident_b)
            if t == 3:
                tb = c["tb"]
                P.dma("act", lambda e: e.dma_start(out=kb.ap[outT_name][:, :, 512 * tb:512 * (tb + 1)].rearrange("c p t -> p c t"), in_=oT[:]), ost, reads=[oT_b], writes=[kb.db[outT_name][tb]])

        nxt_xT = load_xT(0)
        xr, xrs = load_x(0)
        pend = None
        for tb in range(NB):
            xT, xT_b = nxt_xT
            c = dict(tb=tb)
            pump(kb, n=12, gate=[xT_b])
            for n in range(16):
                wt, wb_, wl, _ = w_r.next()
                P.dma("sp", lambda e: e.dma_start(out=wt[:], in_=wu[:, :, 512 * n:512 * (n + 1)]), wl, reads=wu_b, writes=[wb_])
                if pend is not None and n < 4:
                    epi_ln(pend, n)
                for m in range(4):
                    ps, psb = kb.bank(2, 4)
                    for kc in range(16):
                        P.op("pe", lambda e, kc=kc: e.matmul(ps[:], lhsT=wt[:, kc, 128 * m:128 * (m + 1)], rhs=xT[:, kc, :], start=(kc == 0), stop=(kc == 15)),
                             reads=[xT_b, wb_], writes=[psb], pe_accum=True)
                    hc = 4 * n + m
                    tmp, tmp_b, _, _ = tmp_r.next()
                    if m % 2 == 0:
                        P.op("act", lambda e: e.activation(out=tmp[:], in_=ps[:], func=AF.Relu), reads=[psb], writes=[tmp_b])
                        P.op("pool", lambda e: e.tensor_tensor(out=hT[:, hc, :], in0=tmp[:], in1=tmp[:], op=ALU.mult), reads=[tmp_b], writes=[hT_b[hc]])
                    else:
                        P.op("dve", lambda e: e.tensor_scalar_max(out=tmp[:], in0=ps[:], scalar1=0.0), reads=[psb], writes=[tmp_b])
                        P.op("act", lambda e: e.activation(out=hT[:, hc, :], in_=tmp[:], func=AF.Square), reads=[tmp_b], writes=[hT_b[hc]])
                if pend is not None and 1 <= n < 5:
                    epi_tr(pend, n - 1)
                if pend is not None and n == 12:
                    xr, xrs = load_x(tb)
            c["xr"], c["xrs"] = xr, xrs
            for n in range(4):
                banks = [(kb.ps[4 + t], kb.pb[4 + t]) for t in range(4)]
                for pc in range(4):
                    wt, wb_, wl, _ = w_r.next()
                    P.dma("sp", lambda e: e.dma_start(out=wt[:], in_=wd[:, 16 * pc:16 * (pc + 1), 512 * n:512 * (n + 1)]), wl, reads=wd_b, writes=[wb_])
                    if n == 0 and pc == 2 and tb + 1 < NB:
                        nxt_xT = load_xT(tb + 1)
                    for t in range(4):
                        ps, psb = banks[t]
                        for kl in range(16):
                            kc = 16 * pc + kl
                            P.op("pe", lambda e, kc=kc, kl=kl: e.matmul(ps[:], lhsT=hT[:, kc, 128 * t:128 * (t + 1)], rhs=wt[:, kl, :], start=(kc == 0), stop=(kc == 63)),
                                 reads=[hT_b[kc], wb_], writes=[psb], pe_accum=True)
                for t in range(4):
                    ps, psb = banks[t]
                    sl = xr[:, t, 512 * n:512 * (n + 1)]
                    P.op("dve", lambda e: e.scalar_tensor_tensor(out=sl, in0=sl, scalar=ALPHA, in1=ps[:], op0=ALU.mult, op1=ALU.add), reads=[psb, tbs[t][n]], writes=[tbs[t][n]])
            pend = c
        for t in range(4):
            epi_ln(pend, t)
            epi_tr(pend, t)


def phase_E(kb, layer, in_name, inT_name, out_name):
    P = kb.P
    wg = kb.ap[f"wb_gate{layer}"].rearrange("(kc p) n -> p kc n", p=128)
    wg_b = need_w(kb, f"wb_gate{layer}")
    wp_d = kb.ap[f"wb_ple{layer}"].rearrange("(kc p) n -> p kc n", p=128)
    with kb.phase():
        C = load_consts(kb)
        ident, ident_b = C["ident_bf"]
        wp = kb.sb("wp", [128, 2, D], BF16)
        wp_b = P.buf()
        P.dma("sp", lambda e: e.dma_start(out=wp[:], in_=wp_d), P.dsem(), reads=need_w(kb, f"wb_ple{layer}"), writes=[wp_b])
        xT_r = Ring(kb, "xT", 2, [128, 16, 512], BF16)
        xr_r = Ring(kb, "xr", 2, [128, 4, D], F32)
        xr_tb = [[P.buf() for _ in range(4)] for _ in range(2)]
        wres = kb.sb("wres", [128, 16, D], BF16)
        wres_b = [P.buf() for _ in range(4)]
        wds = P.dsem()
        for n in range(4):
            P.dma("sp", lambda e: e.dma_start(out=wres[:, :, 512 * n:512 * (n + 1)], in_=wg[:, :, 512 * n:512 * (n + 1)]), wds, reads=wg_b, writes=[wres_b[n]])
        ps_r = Ring(kb, "pst", 2, [128, 256], F32)
        xb_r = Ring(kb, "xb", 2, [128, 256], BF16)
        pT_r = Ring(kb, "pT", 2, [128, 2, 512], BF16)
        tmp_r = Ring(kb, "tmp", 3, [128, 512], F32)

        def load_blk(tb):
            xT, xT_b, xl, _ = xT_r.next()
            xr, _, xrl, _ = xr_r.next()
            tbs = xr_tb[xr_r.i % 2]
            P.dma("sp", lambda e: e.dma_start(out=xT[:], in_=kb.ap[inT_name][:, :, 512 * tb:512 * (tb + 1)].rearrange("c p t -> p c t")), xl, reads=[kb.db[inT_name][tb]], writes=[xT_b])
            P.dma("sp", lambda e: e.dma_start(out=xr[:], in_=kb.ap[in_name][512 * tb:512 * (tb + 1), :].rearrange("(t p) d -> p t d", p=128)), xrl, reads=[kb.db[in_name][tb]], writes=tbs)
            pT, pT_b, _, _ = pT_r.next()
            for t in range(4):
                pst, pst_b, pl, _ = ps_r.next()
                r0 = 512 * tb + 128 * t
                P.dma("sp", lambda e: e.dma_start(out=pst[:], in_=kb.ap["p"][layer, r0:r0 + 128, :]), pl, writes=[pst_b])
                cast_transpose(kb, pst[:], pst_b, 256, xb_r, pT, pT_b, 128 * t, ident[:], ident_b)
            return xT, xT_b, xr, tbs, pT, pT_b

        nxt = load_blk(0)
        for tb in range(NB):
            xT, xT_b, xr, tbs, pT, pT_b = nxt
            for n in range(4):
                wt, wb_ = wres[:, :, 512 * n:512 * (n + 1)], wres_b[n]
                if n == 1 and tb + 1 < NB:
                    nxt = load_blk(tb + 1)
                for t in range(4):
                    G, G_b = kb.bank(2, 5)
                    E, E_b = kb.bank(5, 8)
                    for kc in range(16):
                        P.op("pe", lambda e, kc=kc: e.matmul(G[:], lhsT=xT[:, kc, 128 * t:128 * (t + 1)], rhs=wt[:, kc, :], start=(kc == 0), stop=(kc == 15)),
                             reads=[xT_b, wb_], writes=[G_b], pe_accum=True)
                    for kc in range(2):
                        P.op("pe", lambda e, kc=kc: e.matmul(E[:], lhsT=pT[:, kc, 128 * t:128 * (t + 1)], rhs=wp[:, kc, 512 * n:512 * (n + 1)], start=(kc == 0), stop=(kc == 1)),
                             reads=[pT_b, wp_b], writes=[E_b], pe_accum=True)
                    tmp, tmp_b, _, _ = tmp_r.next()
                    sl = xr[:, t, 512 * n:512 * (n + 1)]
                    P.op("act", lambda e: e.activation(out=tmp[:], in_=G[:], func=AF.Sigmoid), reads=[G_b], writes=[tmp_b])
                    P.op("dve", lambda e: e.tensor_tensor(out=tmp[:], in0=tmp[:], in1=E[:], op=ALU.mult), reads=[tmp_b, E_b], writes=[tmp_b])
                    P.op("pool", lambda e: e.tensor_tensor(out=sl, in0=sl, in1=tmp[:], op=ALU.add), reads=[tmp_b, tbs[t]], writes=[tbs[t]])
            P.dma("act", lambda e: e.dma_start(out=kb.ap[out_name][512 * tb:512 * (tb + 1), :].rearrange("(t p) d -> p t d", p=128), in_=xr[:]), xr_r.st[xr_r.i % 2], reads=tbs, writes=[kb.db[out_name][tb]])


def build(n_layers=2, dbg=(), stop=None):
    kb = KB(dbg)
    nc, P = kb.nc, kb.P
    x = kb.din("x", (S, D))
    kb.db["x"] = [P.buf() for _ in range(NB)]
    kb.din("p", (2, S, 256))
    for nm, shp in [("ev_w_in", (1, D, EV_IN)), ("ev_b_fgate", (1, 8)), ("ev_dw_kernel", (1, 31, 1024)), ("ev_dw_bias", (1, 1024)),
                    ("ev_cnorm_g", (1, 1024)), ("ev_cnorm_b", (1, 1024)), ("ev_w_out", (1, D, D)), ("od_w_in", (1, D, OD_IN)),
                    ("od_b_igate", (1, 8)), ("od_b_fgate", (1, 8)), ("od_w_out", (1, D, D)), ("ln_mix_g", (2, D)), ("ln_mix_b", (2, D)),
                    ("w_up", (2, D, DFF)), ("w_down", (2, DFF, D)), ("ln_ffn_g", (2, D)), ("ln_ffn_b", (2, D)),
                    ("w_ple", (2, 256, D)), ("w_ple_gate", (2, D, D))]:
        kb.din(nm, shp)
    for nm, dt in (("ident_bf", BF16), ("tri_bf", BF16), ("ones_bf", BF16), ("tri_f", F32), ("ones_f", F32), ("ident_f", F32)):
        kb.din(nm, (128, 128), dt)
    kb.dscr("qT", (8, 128, S), BF16)
    kb.dscr("kT", (8, 128, S), BF16)
    kb.dscr("v", (S, D), BF16)
    kb.dscr("g", (8, 128, S), BF16)
    kb.dscr("gates", (S, 16), F32, nbuf=1)
    kb.dscr("oT", (16, 128, S), BF16)
    kb.dscr("co", (8, 128, S), F32)
    kb.dscr("mixT", (16, 128, S), BF16)
    kb.dscr("x1", (S, D), F32)
    kb.dscr("x1T", (16, 128, S), BF16)
    kb.dscr("x2", (S, D), F32)
    kb.dscr("x2T", (16, 128, S), BF16)
    kb.dscr("xn", (S, D), F32)
    kb.dscr("y", (S, D), F32, out=True)

    def conv_layer(l):
        if l == 0:
            convert_weight(kb, kb.ap["ev_w_in"][0], "wb_ev_in", (D, EV_IN))
            convert_weight(kb, kb.ap["ev_w_out"][0], "wb_ev_out", (D, D))
        else:
            convert_weight(kb, kb.ap["od_w_in"][0], "wb_od_in", (D, OD_IN))
            convert_weight(kb, kb.ap["od_w_out"][0], "wb_od_out", (D, D))
        convert_weight(kb, kb.ap["w_up"][l], f"wb_up{l}", (D, DFF))
        convert_weight(kb, kb.ap["w_down"][l], f"wb_down{l}", (DFF, D))
        convert_weight(kb, kb.ap["w_ple_gate"][l], f"wb_gate{l}", (D, D))
        convert_weight(kb, kb.ap["w_ple"][l], f"wb_ple{l}", (256, D))

    conv_layer(0)
    steps = []
    for l in range(n_layers):
        last_l = (l == n_layers - 1)
        xin = "x" if l == 0 else "xn"
        if l == 0:
            steps.append(("A0", lambda: phase_A(kb, 0, (kb.ap["x"], kb.db["x"]))))
            if n_layers > 1:
                steps.append(("cv1", lambda: conv_layer(1)))
            steps.append(("B0", lambda: phase_B0(kb)))
            steps.append(("B2", lambda: phase_B2(kb)))
            steps.append(("C0", lambda: phase_C(kb, 0, "x", "wb_ev_out", "ln_mix_g", "ln_mix_b", "x1", "x1T")))
        else:
            steps.append(("A1", lambda: phase_A(kb, 1, (kb.ap["xn"], kb.db["xn"]))))
            steps.append(("B1", lambda: phase_B1(kb)))
            steps.append(("C1", lambda: phase_C(kb, 1, "xn", "wb_od_out", "ln_mix_g", "ln_mix_b", "x1", "x1T")))
        steps.append((f"D{l}", lambda l=l: phase_D(kb, l, "x1", "x1T", "x2", "x2T")))
        steps.append((f"E{l}", lambda l=l, last_l=last_l: phase_E(kb, l, "x2", "x2T", "y" if last_l else "xn")))
    for nm, fn in steps:
        fn()
        if stop == nm:
            break
    finals = list(P.pending_dma)
    if not finals:
        finals = [o for o in P.last.values() if o is not None]
    P.emit(nc, final_wait_ops=finals)
    return kb


def consts():
    i = np.eye(128, dtype=np.float32)
    tri = np.triu(np.ones((128, 128), np.float32))
    one = np.ones((128, 128), np.float32)
    bf = ml_dtypes.bfloat16
    return {"ident_bf": i.astype(bf), "tri_bf": tri.astype(bf), "ones_bf": one.astype(bf), "tri_f": tri, "ones_f": one, "ident_f": i}


_KB = None


def kernel(**inputs):
    global _KB
    if _KB is None:
        _KB = build(n_layers=2)
    kb = _KB
    c = consts()
    x = np.asarray(inputs["x"], dtype=np.float32)
    p = np.asarray(inputs["p"], dtype=np.float32)
    shared = {k: np.ascontiguousarray(np.asarray(v, dtype=np.float32)) for k, v in inputs.items() if k not in ("x", "p")}
    in_maps = []
    for b in range(8):
        m = dict(c)
        m.update(shared)
        m["x"] = np.ascontiguousarray(x[b])
        m["p"] = np.ascontiguousarray(p[:, b])
        in_maps.append(m)
    res = run_bass_kernel_spmd(kb.nc, in_maps, core_ids=list(range(8)))
    return np.stack([np.asarray(r["y"], dtype=np.float32) for r in res.results], axis=0)
```

```python
import contextlib
from contextlib import ExitStack
import numpy as np
import ml_dtypes
import concourse.bass as bass
import concourse.mybir as mybir
from concourse.bass_utils import run_bass_kernel_spmd

F32 = mybir.dt.float32
BF16 = mybir.dt.bfloat16
AF = mybir.ActivationFunctionType
ALU = mybir.AluOpType

ENGS = ("pe", "act", "dve", "pool", "sp")


class Buf:
    __slots__ = ("name", "last_w", "readers")

    def __init__(self, name):
        self.name = name
        self.last_w = None
        self.readers = []


class DSem:
    __slots__ = ("name", "count", "h")

    def __init__(self, name):
        self.name = name
        self.count = 0
        self.h = None


class Op:
    __slots__ = ("eng", "fn", "deps", "needs_inc", "tokval", "dsem", "is_dma", "idx")

    def __init__(self, eng, fn, is_dma=False, dsem=None):
        self.eng = eng
        self.fn = fn
        self.deps = []
        self.needs_inc = False
        self.tokval = None
        self.dsem = dsem
        self.is_dma = is_dma
        self.idx = None


class _Rec:
    def __init__(self):
        self.call = None

    def __getattr__(self, name):
        def f(*a, **kw):
            self.call = (name, a, kw)
        return f


class Prog:
    def __init__(self):
        self.ops = {e: [] for e in ENGS}
        self.dsems = []
        self.free_dsems = []
        self.phase_dsems = []
        self.nbuf = 0
        self.last = {e: None for e in ENGS}
        self.pending_dma = []
        self.nops = 0

    def buf(self, name=None):
        self.nbuf += 1
        return Buf(name or f"b{self.nbuf}")

    def dsem(self, name=None):
        if self.free_dsems:
            d = self.free_dsems.pop()
        else:
            d = DSem(name or f"d{len(self.dsems)}")
            self.dsems.append(d)
        self.phase_dsems.append(d)
        return d

    def _add(self, op, reads, writes, pe_accum=False):
        deps = []
        for b in reads:
            if b.last_w is not None:
                deps.append(b.last_w)
        for b in writes:
            if b.last_w is not None:
                lw = b.last_w
                if not (pe_accum and lw.eng == "pe" and op.eng == "pe" and not lw.is_dma and not op.is_dma):
                    deps.append(lw)
            deps.extend(b.readers)
        seen = set()
        for d in deps:
            if d is op or id(d) in seen:
                continue
            seen.add(id(d))
            op.deps.append(d)
            d.needs_inc = True
        for b in reads:
            b.readers.append(op)
        for b in writes:
            b.last_w = op
            b.readers = []
        op.idx = self.nops
        self.nops += 1
        self.ops[op.eng].append(op)
        if op.is_dma:
            self.pending_dma.append(op)
        else:
            self.last[op.eng] = op
        return op

    @staticmethod
    def _freeze(fn):
        rec = _Rec()
        fn(rec)
        name, a, kw = rec.call
        return lambda h: getattr(h, name)(*a, **kw)

    def op(self, eng, fn, reads=(), writes=(), pe_accum=False):
        return self._add(Op(eng, self._freeze(fn)), list(reads), list(writes), pe_accum)

    def dma(self, queue, fn, dsem, reads=(), writes=(), after=()):
        op = Op(queue, self._freeze(fn), is_dma=True, dsem=dsem)
        dsem.count += 16
        op.tokval = dsem.count
        for b in after:
            if b.last_w is not None and b.last_w not in op.deps:
                op.deps.append(b.last_w)
                b.last_w.needs_inc = True
        return self._add(op, list(reads), list(writes))

    def barrier(self):
        deps = [o for o in self.last.values() if o is not None] + list(self.pending_dma)
        for e in ENGS:
            op = Op(e, None)
            for d in deps:
                op.deps.append(d)
                d.needs_inc = True
            op.idx = self.nops
            self.nops += 1
            self.ops[e].append(op)
        self.pending_dma = []
        self.free_dsems.extend(self.phase_dsems)
        self.phase_dsems = []

    def emit(self, nc, final_wait_ops=()):
        engh = {"pe": nc.tensor, "act": nc.scalar, "dve": nc.vector, "pool": nc.gpsimd, "sp": nc.sync}
        for e in ENGS:
            c = 0
            for op in self.ops[e]:
                if not op.is_dma and op.needs_inc and op.fn is not None:
                    c += 1
                    op.tokval = c
        with ExitStack() as st:
            esem = {e: st.enter_context(nc.semaphore(f"s_{e}")) for e in ENGS}
            for d in self.dsems:
                if d.count > 0:
                    d.h = st.enter_context(nc.semaphore(f"q_{d.name}"))
            nwait = 0
            for e in ENGS:
                h = engh[e]
                known = {}
                for op in self.ops[e]:
                    need = {}
                    for d in op.deps:
                        if d.is_dma:
                            key = ("d", id(d.dsem))
                            semh = d.dsem.h
                        else:
                            key = ("e", d.eng)
                            semh = esem[d.eng]
                        v = d.tokval
                        if known.get(key, 0) >= v:
                            continue
                        if key not in need or need[key][1] < v:
                            need[key] = (semh, v)
                    for key, (semh, v) in need.items():
                        h.wait_ge(semh, v)
                        known[key] = v
                        nwait += 1
                    if op.fn is None:
                        continue
                    ins = op.fn(h)
                    if op.is_dma:
                        ins.then_inc(op.dsem.h, 16)
                    elif op.needs_inc:
                        ins.then_inc(esem[e], 1)
            for op in final_wait_ops:
                if op.is_dma:
                    nc.sync.wait_ge(op.dsem.h, op.tokval)
                else:
                    nc.sync.wait_ge(esem[op.eng], op.tokval)
            self.nwait = nwait


S = 4096
D = 2048
NT = S // 128
NB = S // 512
DFF = 8192
ALPHA = float(4 ** 0.25)
EPS = 1e-5
EV_IN = 5128
OD_IN = 6160

WEIGHTS = [
    ("ev_w_in", (D, EV_IN)), ("ev_w_out", (D, D)), ("od_w_in", (D, OD_IN)), ("od_w_out", (D, D)),
]


class Ring:
    def __init__(self, kb, name, n, shape, dt):
        self.t = [kb.sb(f"{name}{i}", shape, dt) for i in range(n)]
        self.b = [kb.P.buf(f"{name}{i}") for i in range(n)]
        self.ld = [kb.P.dsem() for i in range(n)]
        self.st = [kb.P.dsem() for i in range(n)]
        self.i = -1
        self.n = n

    def next(self):
        self.i += 1
        j = self.i % self.n
        return self.t[j], self.b[j], self.ld[j], self.st[j]


class KB:
    def __init__(self, dbg=()):
        self.nc = bass.Bass("TRN2", target_bir_lowering=False)
        self.P = Prog()
        self.dbg = set(dbg)
        self.ap = {}
        self.db = {}
        self.uid = 0
        self.st = None
        self.flip = 0
        self.cq = []
        self.cpending = {}

    def din(self, name, shape, dt=F32):
        self.ap[name] = self.nc.dram_tensor(name, list(shape), dt, kind="ExternalInput").ap()
        return self.ap[name]

    def dscr(self, name, shape, dt, nbuf=NB, out=False):
        kind = "ExternalOutput" if (out or name in self.dbg) else "Internal"
        self.ap[name] = self.nc.dram_tensor(name, list(shape), dt, kind=kind).ap()
        self.db[name] = [self.P.buf(f"{name}.{i}") for i in range(nbuf)]
        return self.ap[name]

    def sb(self, name, shape, dt):
        self.uid += 1
        return self.st.enter_context(self.nc.sbuf_tensor(f"{name}_{self.uid}", list(shape), dt))

    @contextlib.contextmanager
    def phase(self):
        with ExitStack() as st:
            self.st = st
            self.uid += 1
            self.ps = [st.enter_context(self.nc.psum_tensor(f"ps{i}_{self.uid}", [128, 512], F32)) for i in range(8)]
            self.pb = [self.P.buf(f"ps{i}") for i in range(8)]
            self.psi = -1
            yield
            self.P.barrier()
        self.st = None

    def bank(self, lo=0, hi=8):
        self.psi += 1
        j = lo + self.psi % (hi - lo)
        return self.ps[j], self.pb[j]

    def alt(self):
        self.flip ^= 1
        return self.flip


def convert_weight(kb, src_ap, name, shape):
    P = kb.P
    n = int(np.prod(shape))
    C = 2048
    rows = n // C
    assert rows * C == n
    dst = kb.nc.dram_tensor(name, list(shape), BF16, kind="Internal").ap()
    kb.ap[name] = dst
    sf = src_ap.rearrange("k n -> (k n)").rearrange("(a c) -> a c", c=C)
    df = dst.rearrange("k n -> (k n)").rearrange("(a c) -> a c", c=C)
    ds = P.dsem()
    P.phase_dsems.remove(ds)
    kb.db[name] = []
    kb.cpending[name] = 0
    R = 256
    for r0 in range(0, rows, R):
        r1 = min(rows, r0 + R)
        kb.cq.append((name, sf[r0:r1, :], df[r0:r1, :], ds))
        kb.cpending[name] += 1


def pump(kb, n=None, gate=(), upto=None):
    P = kb.P
    k = 0
    while kb.cq:
        if upto is not None:
            if kb.cpending.get(upto, 0) == 0:
                break
        elif n is not None and k >= n:
            break
        name, s_, d_, ds = kb.cq.pop(0)
        b = P.buf()
        P.dma("pool", lambda e: e.dma_start(out=d_, in_=s_), ds, after=list(gate), writes=[b])
        kb.db[name].append(b)
        kb.cpending[name] -= 1
        k += 1


def need_w(kb, name):
    if kb.cpending.get(name, 0) > 0:
        pump(kb, upto=name)
    return kb.db[name]


def cast_tile(kb, src_t, src_b, W, xb_ring, cast_eng=None):
    P = kb.P
    xb, xb_b, _, _ = xb_ring.next()
    ce = cast_eng or ("act" if kb.alt() else "dve")
    if ce == "act":
        P.op("act", lambda e: e.copy(out=xb[:, :W], in_=src_t), reads=[src_b], writes=[xb_b])
    elif ce == "pool":
        P.op("pool", lambda e: e.tensor_copy(out=xb[:, :W], in_=src_t), reads=[src_b], writes=[xb_b])
    else:
        P.op("dve", lambda e: e.tensor_copy(out=xb[:, :W], in_=src_t), reads=[src_b], writes=[xb_b])
    return xb, xb_b


def transpose_tile(kb, xb, xb_b, W, dstT, dstT_b, col0, ident, ident_b):
    P = kb.P
    nk = W // 128
    for g0 in range(0, nk, 4):
        ng = min(4, nk - g0)
        ps, psb = kb.bank(0, 2)
        pv = ps.bitcast(BF16)
        for a in range(ng):
            kc = g0 + a
            P.op("pe", lambda e, a=a, kc=kc, pv=pv: e.transpose(out=pv[:, 128 * a:128 * (a + 1)], in_=xb[:, 128 * kc:128 * (kc + 1)], identity=ident),
                 reads=[xb_b, ident_b], writes=[psb], pe_accum=True)
        src = pv[:, 0:128 * ng].rearrange("p (a c) -> p a c", c=128)
        dst = dstT[:, g0:g0 + ng, col0:col0 + 128]
        if kb.alt():
            P.op("act", lambda e, src=src, dst=dst: e.copy(out=dst, in_=src), reads=[psb], writes=[dstT_b])
        else:
            P.op("dve", lambda e, src=src, dst=dst: e.tensor_copy(out=dst, in_=src), reads=[psb], writes=[dstT_b])


def cast_transpose(kb, src_t, src_b, W, xb_ring, dstT, dstT_b, col0, ident, ident_b, cast_eng=None):
    xb, xb_b = cast_tile(kb, src_t, src_b, W, xb_ring, cast_eng)
    transpose_tile(kb, xb, xb_b, W, dstT, dstT_b, col0, ident, ident_b)


def load_consts(kb):
    P = kb.P
    c = {}
    for nm, dt in (("ident_bf", BF16), ("tri_bf", BF16), ("ones_bf", BF16), ("tri_f", F32), ("ones_f", F32), ("ident_f", F32)):
        t = kb.sb(nm, [128, 128], dt)
        b = P.buf(nm)
        ds = P.dsem()
        P.dma("sp", lambda e, t=t, nm=nm: e.dma_start(out=t[:], in_=kb.ap[nm]), ds, writes=[b])
        c[nm] = (t, b)
    return c


def phase_A(kb, layer, xsrc):
    P, nc = kb.P, kb.nc
    ev = layer == 0
    wname = "wb_ev_in" if ev else "wb_od_in"
    NIN = EV_IN if ev else OD_IN
    wv = kb.ap[wname].rearrange("(kc p) n -> p kc n", p=128)
    wbufs = need_w(kb, wname)
    ng = 8 if ev else 16
    gcol = 3072 if ev else 4096
    with kb.phase():
        C = load_consts(kb)
        ident, ident_b = C["ident_bf"]
        xs_r = Ring(kb, "xs", 2, [128, D], F32)
        xb_r = Ring(kb, "xb", 2, [128, D], BF16)
        xT_r = Ring(kb, "xT", 2, [128, 16, 512], BF16)
        w_r = Ring(kb, "wst", 3, [128, 16, 512], BF16)
        o16_r = Ring(kb, "o16", 3, [128, 4, 512], BF16)
        o32_r = Ring(kb, "o32", 2, [128, 4, 512], F32)
        sg_r = Ring(kb, "sg", 2, [128, 512], F32)
        wg = kb.sb("wg", [128, 16, 16], BF16)
        wg_b = P.buf("wg")
        gt = kb.sb("gt", [128, NT, 16], F32)
        gt_b = P.buf("gt")
        gbias = kb.sb("gbias", [128, 16], F32)
        gbias_b = P.buf("gbias")
        P.dma("sp", lambda e: e.dma_start(out=wg[:, :, :ng], in_=wv[:, :, gcol:gcol + ng]), P.dsem(), reads=wbufs, writes=[wg_b])
        if ev:
            P.dma("sp", lambda e: e.dma_start(out=gbias[:, 0:8], in_=kb.ap["ev_b_fgate"][0:1, :].partition_broadcast(128)), P.dsem(), writes=[gbias_b])
        else:
            d_ = P.dsem()
            P.dma("sp", lambda e: e.dma_start(out=gbias[:, 0:8], in_=kb.ap["od_b_igate"][0:1, :].partition_broadcast(128)), d_, writes=[gbias_b])
            P.dma("sp", lambda e: e.dma_start(out=gbias[:, 8:16], in_=kb.ap["od_b_fgate"][0:1, :].partition_broadcast(128)), d_, writes=[gbias_b])

        if ev:
            groups = [("fm", "qT", 0, 0), ("fm", "qT", 512, 4), ("fm", "kT", 1024, 0), ("fm", "kT", 1536, 4),
                      ("tm", "v", 2048, 0), ("tm", "v", 2560, 512),
                      ("glu", "g", 3080, 0), ("glu", "g", 3080 + 512, 4)]
        else:
            groups = [("fm", "qT", 0, 0), ("fm", "qT", 512, 4), ("fm", "kT", 1024, 0), ("fm", "kT", 1536, 4)]
            groups += [("tm", "v", 2048 + 512 * i, 512 * i) for i in range(4)]
            groups += [("sig", "oT", 4112 + 512 * i, 4 * i) for i in range(4)]

        def load_w(c0, width=512):
            wt, wb_, wl, _ = w_r.next()
            P.dma("sp", lambda e: e.dma_start(out=wt[:, :, :width], in_=wv[:, :, c0:c0 + width]), wl, reads=wbufs, writes=[wb_])
            return wt, wb_

        for tb in range(NB):
            xT, xT_b, _, _ = xT_r.next()
            for t in range(4):
                xs, xs_b, xl, _ = xs_r.next()
                r0 = tb * 512 + t * 128
                P.dma("sp", lambda e, xs=xs, r0=r0: e.dma_start(out=xs[:], in_=xsrc[0][r0:r0 + 128, :]), xl, reads=[xsrc[1][tb]], writes=[xs_b])
                cast_transpose(kb, xs[:], xs_b, D, xb_r, xT, xT_b, 128 * t, ident[:], ident_b)
            pump(kb, n=3 if ev else 0, gate=[xT_b])
            for t in range(4):
                ps, psb = kb.bank(2, 8)
                for kc in range(16):
                    P.op("pe", lambda e, ps=ps, kc=kc, t=t, xT=xT: e.matmul(ps[:, 0:ng], lhsT=xT[:, kc, 128 * t:128 * (t + 1)], rhs=wg[:, kc, 0:ng], start=(kc == 0), stop=(kc == 15)),
                         reads=[xT_b, wg_b], writes=[psb], pe_accum=True)
                tt = tb * 4 + t
                P.op("dve", lambda e, ps=ps, tt=tt: e.tensor_tensor(out=gt[:, tt, 0:ng], in0=ps[:, 0:ng], in1=gbias[:, 0:ng], op=ALU.add),
                     reads=[psb, gbias_b], writes=[gt_b])
            for kind, dst, c0, d0 in groups:
                if kind == "fm" or kind == "sig":
                    wt, wb_ = load_w(c0)
                    o, o_b, _, ost = o16_r.next()
                    for m in range(4):
                        ps, psb = kb.bank(2, 8)
                        for kc in range(16):
                            P.op("pe", lambda e, ps=ps, kc=kc, m=m, wt=wt, xT=xT: e.matmul(ps[:], lhsT=wt[:, kc, 128 * m:128 * (m + 1)], rhs=xT[:, kc, :], start=(kc == 0), stop=(kc == 15)),
                                 reads=[xT_b, wb_], writes=[psb], pe_accum=True)
                        if kind == "sig":
                            P.op("act", lambda e, ps=ps, o=o, m=m: e.activation(out=o[:, m, :], in_=ps[:], func=AF.Sigmoid), reads=[psb], writes=[o_b])
                        elif dst == "kT" and not ev:
                            P.op("act", lambda e, ps=ps, o=o, m=m: e.mul(out=o[:, m, :], in_=ps[:], mul=float(128 ** -0.5)), reads=[psb], writes=[o_b])
                        elif kb.alt():
                            P.op("act", lambda e, ps=ps, o=o, m=m: e.copy(out=o[:, m, :], in_=ps[:]), reads=[psb], writes=[o_b])
                        else:
                            P.op("dve", lambda e, ps=ps, o=o, m=m: e.tensor_copy(out=o[:, m, :], in_=ps[:]), reads=[psb], writes=[o_b])
                    dap = kb.ap[dst][d0:d0 + 4, :, tb * 512:(tb + 1) * 512].rearrange("h p t -> p h t")
                    P.dma("act", lambda e, o=o, dap=dap: e.dma_start(out=dap, in_=o[:]), ost, reads=[o_b], writes=[kb.db[dst][tb]])
                elif kind == "tm":
                    wt, wb_ = load_w(c0)
                    o, o_b, _, ost = o16_r.next()
                    for t in range(4):
                        ps, psb = kb.bank(2, 8)
                        for kc in range(16):
                            P.op("pe", lambda e, ps=ps, kc=kc, t=t, wt=wt, xT=xT: e.matmul(ps[:], lhsT=xT[:, kc, 128 * t:128 * (t + 1)], rhs=wt[:, kc, :], start=(kc == 0), stop=(kc == 15)),
                                 reads=[xT_b, wb_], writes=[psb], pe_accum=True)
                        if kb.alt():
                            P.op("act", lambda e, ps=ps, o=o, t=t: e.copy(out=o[:, t, :], in_=ps[:]), reads=[psb], writes=[o_b])
                        else:
                            P.op("dve", lambda e, ps=ps, o=o, t=t: e.tensor_copy(out=o[:, t, :], in_=ps[:]), reads=[psb], writes=[o_b])
                    dap = kb.ap[dst][tb * 512:(tb + 1) * 512, d0:d0 + 512].rearrange("(t p) c -> p t c", p=128)
                    P.dma("act", lambda e, o=o, dap=dap: e.dma_start(out=dap, in_=o[:]), ost, reads=[o_b], writes=[kb.db[dst][tb]])
                elif kind == "glu":
                    wa, wa_b = load_w(c0)
                    wgt, wgt_b = load_w(c0 + 1024)
                    o, o_b, _, ost = o16_r.next()
                    for m in range(4):
                        pa, pa_b = kb.bank(2, 8)
                        pg, pg_b = kb.bank(2, 8)
                        for kc in range(16):
                            P.op("pe", lambda e, pg=pg, kc=kc, m=m, wgt=wgt, xT=xT: e.matmul(pg[:], lhsT=wgt[:, kc, 128 * m:128 * (m + 1)], rhs=xT[:, kc, :], start=(kc == 0), stop=(kc == 15)),
                                 reads=[xT_b, wgt_b], writes=[pg_b], pe_accum=True)
                        for kc in range(16):
                            P.op("pe", lambda e, pa=pa, kc=kc, m=m, wa=wa, xT=xT: e.matmul(pa[:], lhsT=wa[:, kc, 128 * m:128 * (m + 1)], rhs=xT[:, kc, :], start=(kc == 0), stop=(kc == 15)),
                                 reads=[xT_b, wa_b], writes=[pa_b], pe_accum=True)
                        sg, sg_b, _, _ = sg_r.next()
                        P.op("act", lambda e, pg=pg, sg=sg: e.activation(out=sg[:], in_=pg[:], func=AF.Sigmoid), reads=[pg_b], writes=[sg_b])
                        P.op("dve", lambda e, pa=pa, sg=sg, o=o, m=m: e.tensor_tensor(out=o[:, m, :], in0=pa[:], in1=sg[:], op=ALU.mult), reads=[pa_b, sg_b], writes=[o_b])
                    dap = kb.ap["g"][d0:d0 + 4, :, tb * 512:(tb + 1) * 512].rearrange("h p t -> p h t")
                    P.dma("act", lambda e, o=o, dap=dap: e.dma_start(out=dap, in_=o[:]), ost, reads=[o_b], writes=[kb.db["g"][tb]])
        P.dma("act", lambda e: e.dma_start(out=kb.ap["gates"].rearrange("(t p) g -> p t g", p=128), in_=gt[:]), P.dsem(), reads=[gt_b], writes=kb.db["gates"])


def load_cw(kb, C, lo=6, hi=8):
    P = kb.P
    cwr = kb.sb("cwr", [34, 1024], F32)
    cwr_b = P.buf("cwr")
    ds = P.dsem()
    P.dma("sp", lambda e: e.dma_start(out=cwr[0:31, :], in_=kb.ap["ev_dw_kernel"][0]), ds, writes=[cwr_b])
    P.dma("sp", lambda e: e.dma_start(out=cwr[31:32, :], in_=kb.ap["ev_dw_bias"][0:1, :]), ds, writes=[cwr_b])
    P.dma("sp", lambda e: e.dma_start(out=cwr[32:33, :], in_=kb.ap["ev_cnorm_g"][0:1, :]), ds, writes=[cwr_b])
    P.dma("sp", lambda e: e.dma_start(out=cwr[33:34, :], in_=kb.ap["ev_cnorm_b"][0:1, :]), ds, writes=[cwr_b])
    cw = kb.sb("cw", [128, 8, 34], F32)
    cw_b = P.buf("cw")
    idf, idf_b = C["ident_f"]
    for c in range(8):
        ps, psb = kb.bank(lo, hi)
        P.op("pe", lambda e, ps=ps, c=c: e.transpose(out=ps[:, 0:34], in_=cwr[0:34, 128 * c:128 * (c + 1)], identity=idf[0:34, 0:34]),
             reads=[cwr_b, idf_b], writes=[psb])
        P.op("dve", lambda e, ps=ps, c=c: e.tensor_copy(out=cw[:, c, :], in_=ps[:, 0:34]), reads=[psb], writes=[cw_b])
    return cw, cw_b


def cum_table(kb, C, gt, gt_b, col0):
    P = kb.P
    tri, tri_b = C["tri_f"]
    one, one_b = C["ones_f"]
    lf = kb.sb("lf", [128, NT, 8], F32)
    lf_b = P.buf("lf")
    P.op("act", lambda e: e.activation(out=lf[:], in_=gt[:, :, col0:col0 + 8], func=AF.Sigmoid), reads=[gt_b], writes=[lf_b])
    P.op("act", lambda e: e.activation(out=lf[:], in_=lf[:], func=AF.Ln), reads=[lf_b], writes=[lf_b])
    lf2 = lf[:].rearrange("p t h -> p (t h)")
    cs, cs_b = kb.bank(6, 8)
    tp, tp_b = kb.bank(6, 8)
    P.op("pe", lambda e: e.matmul(cs[:, 0:256], lhsT=tri[:], rhs=lf2, start=True, stop=True), reads=[lf_b, tri_b], writes=[cs_b])
    P.op("pe", lambda e: e.matmul(tp[:, 0:256], lhsT=one[:], rhs=lf2, start=True, stop=True), reads=[lf_b, one_b], writes=[tp_b])
    tot = kb.sb("tot", [128, NT, 8], F32)
    tot_b = P.buf("tot")
    pin = kb.sb("pin", [128, NT, 8], F32)
    pin_b = P.buf("pin")
    Ft = kb.sb("Ft", [128, NT, 8], F32)
    Ft_b = P.buf("Ft")
    P.op("dve", lambda e: e.tensor_copy(out=tot[:].rearrange("p t h -> p (t h)"), in_=tp[:, 0:256]), reads=[tp_b], writes=[tot_b])
    for h in range(8):
        P.op("dve", lambda e, h=h: e.tensor_tensor_scan(out=pin[:, :, h], data0=one[:, 0:NT], data1=tot[:, :, h], initial=0.0, op0=ALU.mult, op1=ALU.add),
             reads=[tot_b, one_b], writes=[pin_b])
    P.op("dve", lambda e: e.tensor_tensor(out=Ft[:].rearrange("p t h -> p (t h)"), in0=cs[:, 0:256], in1=pin[:].rearrange("p t h -> p (t h)"), op=ALU.add),
         reads=[cs_b, pin_b], writes=[Ft_b])
    P.op("dve", lambda e: e.tensor_tensor(out=Ft[:], in0=Ft[:], in1=tot[:], op=ALU.subtract), reads=[Ft_b, tot_b], writes=[Ft_b])
    return (Ft, Ft_b), (pin, pin_b), (lf, lf_b)


def phase_B0(kb):
    P = kb.P
    sc = float(128 ** -0.5)
    with kb.phase():
        C = load_consts(kb)
        tri_bf, tri_bf_b = C["tri_bf"]
        ones_bf, ones_bf_b = C["ones_bf"]
        cw, cw_b = load_cw(kb, C)
        gt = kb.sb("gt", [128, NT, 16], F32)
        gt_b = P.buf("gt")
        P.dma("sp", lambda e: e.dma_start(out=gt[:], in_=kb.ap["gates"].rearrange("(t p) g -> p t g", p=128)), P.dsem(), reads=kb.db["gates"], writes=[gt_b])
        (Ft, Ft_b), (pin, pin_b), _ = cum_table(kb, C, gt, gt_b, 0)
        q_r = Ring(kb, "q", 2, [128, S], BF16)
        k_r = Ring(kb, "k", 2, [128, S], BF16)
        v_r = Ring(kb, "v", 2, [128, NT, 128], BF16)
        gp_r = Ring(kb, "gp", 2, [128, 32 + S], BF16)
        dgw_r = Ring(kb, "dgw", 2, [128, 31, 128], BF16)
        ident_bf, ident_bf_b = C["ident_bf"]
        bias_r = Ring(kb, "bias", 2, [128, NB, NT], F32)
        pT_r = Ring(kb, "pT", 6, [128, 512], BF16)
        rc_r = Ring(kb, "rc", 2, [128, 512], F32)
        os_r = Ring(kb, "os", 3, [128, 512], BF16)
        acc_r = Ring(kb, "acc", 3, [128, 512], F32)
        for i in range(2):
            P.op("pool", lambda e, i=i: e.memset(gp_r.t[i][:, 0:32], 0.0), writes=[gp_r.b[i]])

        def load_head(h):
            qT, q_b, ql, _ = q_r.next()
            kT, k_b, kl, _ = k_r.next()
            v, v_b, vl, _ = v_r.next()
            gp, gp_b, gl, _ = gp_r.next()
            P.dma("sp", lambda e: e.dma_start(out=kT[:], in_=kb.ap["kT"][h]), kl, reads=kb.db["kT"], writes=[k_b])
            P.dma("sp", lambda e: e.dma_start(out=qT[:], in_=kb.ap["qT"][h]), ql, reads=kb.db["qT"], writes=[q_b])
            P.dma("sp", lambda e: e.dma_start(out=v[:], in_=kb.ap["v"][:, 128 * h:128 * (h + 1)].rearrange("(t p) d -> p t d", p=128)), vl, reads=kb.db["v"], writes=[v_b])
            P.dma("sp", lambda e: e.dma_start(out=gp[:, 32:32 + S], in_=kb.ap["g"][h]), gl, reads=kb.db["g"], writes=[gp_b])
            return (qT, q_b, kT, k_b, v, v_b, gp, gp_b)

        def build_dgw(hh):
            dgw, dgw_b, _, _ = dgw_r.next()
            for tap in range(31):
                P.op("pool", lambda e: e.tensor_scalar(out=dgw[:, tap, :], in0=ident_bf[:], scalar1=cw[:, hh, tap:tap + 1], scalar2=0.0, op0=ALU.mult, op1=ALU.add),
                     reads=[ident_bf_b, cw_b], writes=[dgw_b])
            return dgw, dgw_b

        nxt_head = load_head(0)
        s_i = [0]
        conv_q = []
        for h in range(8):
            qT, q_b, kT, k_b, v, v_b, gp, gp_b = nxt_head
            if h + 1 < 8:
                nxt_head = load_head(h + 1)
            bias, bias_b, _, _ = bias_r.next()
            if h == 0:
                nxt_dgw = build_dgw(0)
            dgw, dgw_b = nxt_dgw
            if h + 1 < 8:
                nxt_dgw = build_dgw(h + 1)
            for j in range(NB):
                P.op("dve", lambda e, j=j, bias=bias: e.tensor_scalar(out=bias[:, j, :], in0=Ft[:, :, h], scalar1=-1.0, scalar2=pin[:, 4 * j + 1, h:h + 1], op0=ALU.mult, op1=ALU.add),
                     reads=[Ft_b, pin_b], writes=[bias_b])
            for j in range(NB):
                nt = 4 * j + 4
                left = (8 - h) * NB - j
                pump(kb, n=(len(kb.cq) + left - 1) // left, gate=[k_b])
                O, O_b = kb.ps[3 + (j % 2)], kb.pb[3 + (j % 2)]
                Dn, Dn_b = kb.ps[5 + (j % 2)], kb.pb[5 + (j % 2)]

                def emitS(t):
                    s_i[0] = (s_i[0] + 1) % 3
                    Sps, S_b = kb.ps[s_i[0]], kb.pb[s_i[0]]
                    c0 = max(0, 128 * (t - 4 * j))
                    P.op("pe", lambda e: e.matmul(Sps[:, c0:512], lhsT=kT[:, 128 * t:128 * (t + 1)], rhs=qT[:, 512 * j + c0:512 * (j + 1)], start=True, stop=True),
                         reads=[k_b, q_b], writes=[S_b])
                    return Sps, S_b, c0

                sq = [emitS(0)]
                if nt > 1:
                    sq.append(emitS(1))
                for t in range(nt):
                    if t + 2 < nt:
                        sq.append(emitS(t + 2))
                    Sps, S_b, c0 = sq.pop(0)
                    pT, pT_b, _, _ = pT_r.next()
                    P.op("act", lambda e, Sps=Sps, c0=c0, pT=pT, t=t: e.activation(out=pT[:, c0:512], in_=Sps[:, c0:512], func=AF.Exp, bias=bias[:, j, t:t + 1], scale=sc),
                         reads=[S_b, bias_b], writes=[pT_b])
                    if t >= 4 * j:
                        P.op("dve", lambda e, c0=c0, pT=pT: e.tensor_tensor(out=pT[:, c0:c0 + 128], in0=pT[:, c0:c0 + 128], in1=tri_bf[:], op=ALU.mult),
                             reads=[pT_b, tri_bf_b], writes=[pT_b])
                    P.op("pe", lambda e, c0=c0, pT=pT, t=t: e.matmul(O[:, c0:512], lhsT=v[:, t, :], rhs=pT[:, c0:512], start=(t == 0), stop=(t == nt - 1)),
                         reads=[v_b, pT_b], writes=[O_b], pe_accum=True)
                    P.op("pe", lambda e, c0=c0, pT=pT, t=t: e.matmul(Dn[:, c0:512], lhsT=ones_bf[:], rhs=pT[:, c0:512], start=(t == 0), stop=(t == nt - 1)),
                         reads=[ones_bf_b, pT_b], writes=[Dn_b], pe_accum=True)
                    for _ in range((31 + nt - 1) // nt):
                        if conv_q:
                            conv_q.pop(0)()
                rc, rc_b, _, _ = rc_r.next()
                os_, os_b, _, ost = os_r.next()
                P.op("dve", lambda e, rc=rc: e.reciprocal(out=rc[:], in_=Dn[:]), reads=[Dn_b], writes=[rc_b])
                P.op("dve", lambda e, rc=rc, os_=os_: e.tensor_tensor(out=os_[:], in0=O[:], in1=rc[:], op=ALU.mult), reads=[O_b, rc_b], writes=[os_b])
                P.dma("sp", lambda e, os_=os_: e.dma_start(out=kb.ap["mixT"][h, :, 512 * j:512 * (j + 1)], in_=os_[:]), ost, reads=[os_b], writes=[kb.db["mixT"][j]])
                while conv_q:
                    conv_q.pop(0)()
                acc, acc_b, _, ast = acc_r.next()
                base = 2 + 512 * j
                cps, cps_b = kb.ps[7], kb.pb[7]

                def mk(tap, cps=cps, cps_b=cps_b, base=base, dgw=dgw, dgw_b=dgw_b, gp=gp, gp_b=gp_b):
                    return lambda: P.op("pe", lambda e: e.matmul(cps[:], lhsT=dgw[:, tap, :], rhs=gp[:, base + tap:base + tap + 512], start=(tap == 0), stop=(tap == 30)),
                                        reads=[dgw_b, gp_b], writes=[cps_b], pe_accum=True)

                def fin(cps=cps, cps_b=cps_b, acc=acc, acc_b=acc_b, ast=ast, h=h, j=j):
                    P.op("act", lambda e: e.activation(out=acc[:], in_=cps[:], func=AF.Identity, bias=cw[:, h, 31:32], scale=1.0), reads=[cps_b, cw_b], writes=[acc_b])
                    P.dma("sp", lambda e: e.dma_start(out=kb.ap["co"][h, :, 512 * j:512 * (j + 1)], in_=acc[:]), ast, reads=[acc_b], writes=[kb.db["co"][j]])

                conv_q.extend([mk(tap) for tap in range(31)] + [fin])
            while conv_q:
                conv_q.pop(0)()


def phase_B2(kb):
    P = kb.P
    with kb.phase():
        C = load_consts(kb)
        one, one_b = C["ones_f"]
        cw, cw_b = load_cw(kb, C)
        co_r = Ring(kb, "co", 2, [128, 8, 512], F32)
        sq_r = Ring(kb, "sq", 2, [128, 8, 512], F32)
        st_r = Ring(kb, "st", 2, [128, 3, 512], F32)
        out_r = Ring(kb, "cvo", 2, [128, 8, 512], BF16)
        for tb in range(NB):
            co, co_b, col, _ = co_r.next()
            P.dma("sp", lambda e, co=co: e.dma_start(out=co[:], in_=kb.ap["co"][:, :, 512 * tb:512 * (tb + 1)].rearrange("c p t -> p c t")), col, reads=[kb.db["co"][tb]], writes=[co_b])
            sq, sq_b, _, _ = sq_r.next()
            P.op("act", lambda e, co=co, sq=sq: e.activation(out=sq[:], in_=co[:], func=AF.Square), reads=[co_b], writes=[sq_b])
            S1, S1_b = kb.bank(0, 6)
            S2, S2_b = kb.bank(0, 6)
            for c in range(8):
                P.op("pe", lambda e, c=c, co=co, S1=S1: e.matmul(S1[:], lhsT=one[:], rhs=co[:, c, :], start=(c == 0), stop=(c == 7)), reads=[co_b, one_b], writes=[S1_b], pe_accum=True)
            for c in range(8):
                P.op("pe", lambda e, c=c, sq=sq, S2=S2: e.matmul(S2[:], lhsT=one[:], rhs=sq[:, c, :], start=(c == 0), stop=(c == 7)), reads=[sq_b, one_b], writes=[S2_b], pe_accum=True)
            stt, st_b, _, _ = st_r.next()
            mean, msq, rstd = stt[:, 0, :], stt[:, 1, :], stt[:, 2, :]
            P.op("act", lambda e, mean=mean, S1=S1: e.mul(out=mean, in_=S1[:], mul=1.0 / 1024), reads=[S1_b], writes=[st_b])
            P.op("dve", lambda e, mean=mean, msq=msq: e.tensor_tensor(out=msq, in0=mean, in1=mean, op=ALU.mult), reads=[st_b], writes=[st_b])
            P.op("dve", lambda e, msq=msq, rstd=rstd, S2=S2: e.scalar_tensor_tensor(out=rstd, in0=S2[:], scalar=1.0 / 1024, in1=msq, op0=ALU.mult, op1=ALU.subtract), reads=[S2_b, st_b], writes=[st_b])
            P.op("dve", lambda e, rstd=rstd: e.tensor_scalar_add(out=rstd, in0=rstd, scalar1=EPS), reads=[st_b], writes=[st_b])
            P.op("act", lambda e, rstd=rstd: e.sqrt(out=rstd, in_=rstd), reads=[st_b], writes=[st_b])
            P.op("dve", lambda e, rstd=rstd: e.reciprocal(out=rstd, in_=rstd), reads=[st_b], writes=[st_b])
            o, o_b, _, ost = out_r.next()
            for c in range(8):
                P.op("dve", lambda e, c=c, co=co, mean=mean: e.tensor_tensor(out=co[:, c, :], in0=co[:, c, :], in1=mean, op=ALU.subtract), reads=[co_b, st_b], writes=[co_b])
                P.op("pool", lambda e, c=c, co=co, rstd=rstd: e.tensor_tensor(out=co[:, c, :], in0=co[:, c, :], in1=rstd, op=ALU.mult), reads=[co_b, st_b], writes=[co_b])
                P.op("act", lambda e, c=c, co=co, o=o: e.activation(out=o[:, c, :], in_=co[:, c, :], func=AF.Silu, scale=cw[:, c, 32:33], bias=cw[:, c, 33:34]), reads=[co_b, cw_b], writes=[o_b])
            P.dma("act", lambda e, o=o: e.dma_start(out=kb.ap["mixT"][8:16, :, 512 * tb:512 * (tb + 1)].rearrange("c p t -> p c t"), in_=o[:]), ost, reads=[o_b], writes=[kb.db["mixT"][tb]])


def phase_B1(kb):
    P = kb.P
    with kb.phase():
        C = load_consts(kb)
        ident, ident_b = C["ident_bf"]
        tri_bf, tri_bf_b = C["tri_bf"]
        ones_bf, ones_bf_b = C["ones_bf"]
        ones_f, ones_f_b = C["ones_f"]
        ident_f, ident_f_b = C["ident_f"]
        gt = kb.sb("gt", [128, NT, 16], F32)
        gt_b = P.buf("gt")
        P.dma("sp", lambda e: e.dma_start(out=gt[:], in_=kb.ap["gates"].rearrange("(t p) g -> p t g", p=128)), P.dsem(), reads=kb.db["gates"], writes=[gt_b])
        (Ft, Ft_b), (pin, pin_b), _ = cum_table(kb, C, gt, gt_b, 8)
        brt = kb.sb("brt", [128, NT, 8], F32)
        brt_b = P.buf("brt")
        aa = kb.sb("aa", [128, NT, 8], F32)
        aa_b = P.buf("aa")
        ee = kb.sb("ee", [128, NB, 8], F32)
        ee_b = P.buf("ee")
        P.op("dve", lambda e: e.memset(brt[:, 0:4, :], 0.0), writes=[brt_b])
        for j in range(1, NB):
            for tl in range(4):
                P.op("dve", lambda e: e.tensor_copy(out=brt[:, 4 * j + tl, :], in_=pin[:, 4 * j - 1, :]), reads=[pin_b], writes=[brt_b])
        P.op("dve", lambda e: e.tensor_tensor(out=aa[:], in0=gt[:, :, 0:8], in1=Ft[:], op=ALU.subtract), reads=[gt_b, Ft_b], writes=[aa_b])
        P.op("dve", lambda e: e.tensor_tensor(out=aa[:], in0=aa[:], in1=brt[:], op=ALU.add), reads=[aa_b, brt_b], writes=[aa_b])
        P.op("act", lambda e: e.activation(out=aa[:], in_=aa[:], func=AF.Exp), reads=[aa_b], writes=[aa_b])
        for j in range(NB):
            P.op("dve", lambda e: e.tensor_tensor(out=ee[:, j, :], in0=pin[:, 4 * j + 3, :], in1=brt[:, 4 * j, :], op=ALU.subtract), reads=[pin_b, brt_b], writes=[ee_b])
        P.op("act", lambda e: e.activation(out=ee[:], in_=ee[:], func=AF.Exp), reads=[ee_b], writes=[ee_b])

        Cst = kb.sb("Cst", [128, 8, 260], F32)
        Cbf = kb.sb("Cbf", [128, 8, 256], BF16)
        nbc = kb.sb("nbc", [128, 8, 128], BF16)
        cst_b = [P.buf() for _ in range(8)]
        cbf_b = [P.buf() for _ in range(8)]
        P.op("pool", lambda e: e.memset(Cst[:], 0.0), writes=cst_b)

        q_r = Ring(kb, "qb", 2, [128, 8, 512], BF16)
        k_r = Ring(kb, "kb", 2, [128, 8, 512], BF16)
        v_r = Ring(kb, "vb", 2, [128, 4, D], BF16)
        o_r = Ring(kb, "ob", 2, [128, 16, 512], BF16)
        ktm_r = Ring(kb, "ktm", 3, [128, 4, 128], BF16)
        vp_r = Ring(kb, "vp", 3, [128, 4, 264], BF16)
        abc_r = Ring(kb, "abc", 2, [128, 8, 4, 128], BF16)
        dg_r = Ring(kb, "dg", 2, [128, 4, 128], F32)
        ei_r = Ring(kb, "ei", 3, [128, 512], F32)
        pT_r = Ring(kb, "pT", 4, [128, 512], BF16)
        dm_r = Ring(kb, "dm", 2, [128, 512], F32)
        t1_r = Ring(kb, "t1", 3, [128, 512], F32)
        ms_r = Ring(kb, "ms", 2, [128, 2, 512], BF16)

        def load_blk(j):
            qb, qb_b, ql, _ = q_r.next()
            kk, kk_b, kl, _ = k_r.next()
            vb, vb_b, vl, _ = v_r.next()
            ob, ob_b, ol, _ = o_r.next()
            sl = slice(512 * j, 512 * (j + 1))
            P.dma("sp", lambda e: e.dma_start(out=kk[:], in_=kb.ap["kT"][:, :, sl].rearrange("h p t -> p h t")), kl, reads=[kb.db["kT"][j]], writes=[kk_b])
            P.dma("sp", lambda e: e.dma_start(out=qb[:], in_=kb.ap["qT"][:, :, sl].rearrange("h p t -> p h t")), ql, reads=[kb.db["qT"][j]], writes=[qb_b])
            P.dma("sp", lambda e: e.dma_start(out=vb[:], in_=kb.ap["v"][sl, :].rearrange("(t p) d -> p t d", p=128)), vl, reads=[kb.db["v"][j]], writes=[vb_b])
            P.dma("sp", lambda e: e.dma_start(out=ob[:], in_=kb.ap["oT"][:, :, sl].rearrange("c p t -> p c t")), ol, reads=[kb.db["oT"][j]], writes=[ob_b])
            return dict(qb=qb, qb_b=qb_b, kk=kk, kk_b=kk_b, vb=vb, vb_b=vb_b, ob=ob, ob_b=ob_b)

        def prep_head(L, j, h):
            c = {}
            kk, kk_b, vb, vb_b = L["kk"], L["kk_b"], L["vb"], L["vb_b"]
            ktm, ktm_b, _, _ = ktm_r.next()
            if j + 1 < NB:
                kt, kt_b = kb.ps[5], kb.pb[5]
                ktv = kt.bitcast(BF16)
                for tl in range(4):
                    P.op("pe", lambda e: e.transpose(out=ktv[:, 128 * tl:128 * (tl + 1)], in_=kk[:, h, 128 * tl:128 * (tl + 1)], identity=ident[:]),
                         reads=[kk_b, ident_b], writes=[kt_b], pe_accum=True)
                P.op("act", lambda e: e.copy(out=ktm[:].rearrange("p a c -> p (a c)"), in_=ktv[:, 0:512]), reads=[kt_b], writes=[ktm_b])
            vp, vp_b, _, _ = vp_r.next()
            for tl in range(4):
                P.op("dve", lambda e: e.tensor_scalar_mul(out=vp[:, tl, 0:256], in0=vb[:, tl, 256 * h:256 * (h + 1)], scalar1=aa[:, 4 * j + tl, h:h + 1]),
                     reads=[vb_b, aa_b], writes=[vp_b])
            P.op("act", lambda e: e.copy(out=vp[:, :, 256], in_=aa[:, 4 * j:4 * j + 4, h]), reads=[aa_b], writes=[vp_b])
            dg, dg_b, _, _ = dg_r.next()
            P.op("pool", lambda e: e.tensor_tensor(out=dg[:], in0=ident_f[:].unsqueeze(1).to_broadcast([128, 4, 128]), in1=Ft[:, 4 * j:4 * j + 4, h:h + 1].to_broadcast([128, 4, 128]), op=ALU.mult),
                 reads=[ident_f_b, Ft_b], writes=[dg_b])
            Bbc, Bbc_b = kb.ps[7], kb.pb[7]
            P.op("pe", lambda e: e.matmul(Bbc[:], lhsT=ones_f[:], rhs=dg[:].rearrange("p a c -> p (a c)"), start=True, stop=True), reads=[ones_f_b, dg_b], writes=[Bbc_b])
            ei, ei_b, _, _ = ei_r.next()
            P.op("act", lambda e: e.activation(out=ei[:], in_=Bbc[:], func=AF.Exp, scale=-1.0, bias=brt[:, 4 * j, h:h + 1]), reads=[Bbc_b, brt_b], writes=[ei_b])
            c.update(ktm=ktm, ktm_b=ktm_b, vp=vp, vp_b=vp_b, ei=ei, ei_b=ei_b)
            return c

        def main_head(L, c, abc, abc_b, j, h):
            qb, qb_b, kk, kk_b, ob, ob_b = L["qb"], L["qb_b"], L["kk"], L["kk_b"], L["ob"], L["ob_b"]
            vp, vp_b, ei, ei_b = c["vp"], c["vp_b"], c["ei"], c["ei_b"]
            O0, O0_b = kb.ps[2], kb.pb[2]
            O1, O1_b = kb.ps[3], kb.pb[3]
            Dn, Dn_b = kb.ps[4], kb.pb[4]
            first = True
            if j > 0:
                P.op("pe", lambda e: e.matmul(O0[:], lhsT=Cbf[:, h, 0:128], rhs=qb[:, h, :], start=True, stop=False), reads=[cbf_b[h], qb_b], writes=[O0_b], pe_accum=True)
                P.op("pe", lambda e: e.matmul(O1[:], lhsT=Cbf[:, h, 128:256], rhs=qb[:, h, :], start=True, stop=False), reads=[cbf_b[h], qb_b], writes=[O1_b], pe_accum=True)
                P.op("pe", lambda e: e.matmul(Dn[:], lhsT=nbc[:, h, :], rhs=qb[:, h, :], start=True, stop=False), reads=[cbf_b[h], qb_b], writes=[Dn_b], pe_accum=True)
                first = False
            for tl in range(4):
                c0 = 128 * tl
                Sps, S_b = kb.bank(0, 2)
                P.op("pe", lambda e: e.matmul(Sps[:, c0:512], lhsT=kk[:, h, 128 * tl:128 * (tl + 1)], rhs=qb[:, h, c0:512], start=True, stop=True), reads=[kk_b, qb_b], writes=[S_b])
                pT, pT_b, _, _ = pT_r.next()
                if c0 + 128 < 512:
                    P.op("act", lambda e: e.copy(out=pT[:, c0 + 128:512], in_=Sps[:, c0 + 128:512]), reads=[S_b], writes=[pT_b])
                P.op("dve", lambda e: e.tensor_tensor(out=pT[:, c0:c0 + 128], in0=Sps[:, c0:c0 + 128], in1=tri_bf[:], op=ALU.mult), reads=[S_b, tri_bf_b], writes=[pT_b])
                st_, sp_ = first, (tl == 3)
                P.op("pe", lambda e: e.matmul(O0[:, c0:512], lhsT=vp[:, tl, 0:128], rhs=pT[:, c0:512], start=st_, stop=sp_), reads=[vp_b, pT_b], writes=[O0_b], pe_accum=True)
                P.op("pe", lambda e: e.matmul(O1[:, c0:512], lhsT=vp[:, tl, 128:256], rhs=pT[:, c0:512], start=st_, stop=sp_), reads=[vp_b, pT_b], writes=[O1_b], pe_accum=True)
                P.op("pe", lambda e: e.matmul(Dn[:, c0:512], lhsT=abc[:, h, tl, :], rhs=pT[:, c0:512], start=st_, stop=sp_), reads=[abc_b, pT_b], writes=[Dn_b], pe_accum=True)
                first = False
            dm, dm_b, _, _ = dm_r.next()
            P.op("act", lambda e: e.activation(out=dm[:], in_=Dn[:], func=AF.Abs), reads=[Dn_b], writes=[dm_b])
            P.op("dve", lambda e: e.tensor_tensor(out=dm[:], in0=dm[:], in1=ei[:], op=ALU.max), reads=[dm_b, ei_b], writes=[dm_b])
            P.op("dve", lambda e: e.reciprocal(out=dm[:], in_=dm[:]), reads=[dm_b], writes=[dm_b])
            ms, ms_b, _, mst = ms_r.next()
            for half, (Oh, Oh_b) in enumerate(((O0, O0_b), (O1, O1_b))):
                t1, t1_b, _, _ = t1_r.next()
                P.op("dve", lambda e: e.tensor_tensor(out=t1[:], in0=Oh[:], in1=dm[:], op=ALU.mult), reads=[Oh_b, dm_b], writes=[t1_b])
                P.op("pool", lambda e: e.tensor_tensor(out=ms[:, half, :], in0=t1[:], in1=ob[:, 2 * h + half, :], op=ALU.mult), reads=[t1_b, ob_b], writes=[ms_b])
            P.dma("pool", lambda e: e.dma_start(out=kb.ap["mixT"][2 * h:2 * h + 2, :, 512 * j:512 * (j + 1)].rearrange("c p t -> p c t"), in_=ms[:]), mst, reads=[ms_b], writes=[kb.db["mixT"][j]])

        def state_head(c, j, h):
            if j + 1 >= NB:
                return
            ktm, ktm_b, vp, vp_b = c["ktm"], c["ktm_b"], c["vp"], c["vp_b"]
            Cps, Cps_b = kb.ps[6], kb.pb[6]
            for tl in range(4):
                P.op("pe", lambda e: e.matmul(Cps[:, 0:257], lhsT=ktm[:, tl, :], rhs=vp[:, tl, 0:257], start=(tl == 0), stop=(tl == 3)), reads=[ktm_b, vp_b], writes=[Cps_b], pe_accum=True)
            P.op("dve", lambda e: e.tensor_scalar_mul(out=Cst[:, h, 0:257], in0=Cst[:, h, 0:257], scalar1=ee[:, j, h:h + 1]), reads=[cst_b[h], ee_b], writes=[cst_b[h]])
            P.op("dve", lambda e: e.scalar_tensor_tensor(out=Cst[:, h, 0:257], in0=Cps[:, 0:257], scalar=ee[:, j, h:h + 1], in1=Cst[:, h, 0:257], op0=ALU.mult, op1=ALU.add),
                 reads=[Cps_b, cst_b[h], ee_b], writes=[cst_b[h]])
            P.op("act", lambda e: e.copy(out=Cbf[:, h, :], in_=Cst[:, h, 0:256]), reads=[cst_b[h]], writes=[cbf_b[h]])
            P.op("pool", lambda e: e.tensor_scalar(out=nbc[:, h, :], in0=ones_bf[:], scalar1=Cst[:, h, 256:257], scalar2=0.0, op0=ALU.mult, op1=ALU.add), reads=[ones_bf_b, cst_b[h]], writes=[cbf_b[h]])

        nxt = load_blk(0)
        for j in range(NB):
            L = nxt
            if j + 1 < NB:
                nxt = load_blk(j + 1)
            abc, abc_b, _, _ = abc_r.next()
            P.op("pool", lambda e: e.tensor_copy(out=abc[:], in_=aa[:, 4 * j:4 * j + 4, :].rearrange("p t h -> p h t").unsqueeze(3).to_broadcast([128, 8, 4, 128])),
                 reads=[aa_b], writes=[abc_b])
            cs = {0: prep_head(L, j, 0)}
            for h in range(8):
                if h + 1 < 8:
                    cs[h + 1] = prep_head(L, j, h + 1)
                main_head(L, cs[h], abc, abc_b, j, h)
                if h > 0:
                    state_head(cs[h - 1], j, h - 1)
            state_head(cs[7], j, 7)


def load_bcast(kb, name, row):
    P = kb.P
    t = kb.sb("bc_" + name, [128, D], F32)
    b = P.buf()
    P.dma("sp", lambda e: e.dma_start(out=t[:], in_=kb.ap[name][row:row + 1, :].partition_broadcast(128)), P.dsem(), writes=[b])
    return t, b


def ln_tile(kb, r, r_b, g, g_b, bb, bb_b, sm_r):
    P = kb.P
    sm, sm_b, _, _ = sm_r.next()
    for c in range(4):
        P.op("dve", lambda e, c=c: e.bn_stats(out=sm[:, 6 * c:6 * c + 6], in_=r[:, 512 * c:512 * (c + 1)]), reads=r_b, writes=[sm_b])
    P.op("dve", lambda e: e.bn_aggr(out=sm[:, 24:26], in_=sm[:, 0:24]), reads=[sm_b], writes=[sm_b])
    P.op("dve", lambda e: e.tensor_scalar_add(out=sm[:, 26:27], in0=sm[:, 25:26], scalar1=EPS), reads=[sm_b], writes=[sm_b])
    P.op("act", lambda e: e.sqrt(out=sm[:, 26:27], in_=sm[:, 26:27]), reads=[sm_b], writes=[sm_b])
    P.op("dve", lambda e: e.reciprocal(out=sm[:, 26:27], in_=sm[:, 26:27]), reads=[sm_b], writes=[sm_b])
    P.op("dve", lambda e: e.scalar_tensor_tensor(out=sm[:, 27:28], in0=sm[:, 24:25], scalar=-1.0, in1=sm[:, 26:27], op0=ALU.mult, op1=ALU.mult), reads=[sm_b], writes=[sm_b])
    P.op("act", lambda e: e.activation(out=r, in_=r, func=AF.Identity, scale=sm[:, 26:27], bias=sm[:, 27:28]), reads=r_b + [sm_b], writes=r_b)
    H = D // 2
    lo_b, hi_b = r_b[:len(r_b) // 2], r_b[len(r_b) // 2:]
    P.op("pool", lambda e: e.tensor_tensor(out=r[:, 0:H], in0=r[:, 0:H], in1=g[:, 0:H], op=ALU.mult), reads=lo_b + [g_b], writes=lo_b)
    P.op("dve", lambda e: e.tensor_tensor(out=r[:, H:D], in0=r[:, H:D], in1=g[:, H:D], op=ALU.mult), reads=hi_b + [g_b], writes=hi_b)
    P.op("dve", lambda e: e.tensor_tensor(out=r[:, 0:H], in0=r[:, 0:H], in1=bb[:, 0:H], op=ALU.add), reads=lo_b + [bb_b], writes=lo_b)
    P.op("pool", lambda e: e.tensor_tensor(out=r[:, H:D], in0=r[:, H:D], in1=bb[:, H:D], op=ALU.add), reads=hi_b + [bb_b], writes=hi_b)


def phase_C(kb, layer, xres_name, wname, ln_g, ln_b, out_name, outT_name):
    P = kb.P
    wv = kb.ap[wname].rearrange("(kc p) n -> p kc n", p=128)
    wbufs = need_w(kb, wname)
    with kb.phase():
        C = load_consts(kb)
        ident, ident_b = C["ident_bf"]
        m_r = Ring(kb, "mT", 2, [128, 16, 512], BF16)
        xr_r = Ring(kb, "xr", 2, [128, 4, D], F32)
        xr_tb = [[[P.buf() for _ in range(4)] for _ in range(4)] for _ in range(2)]
        wres = kb.sb("wres", [128, 16, D], BF16)
        wres_b = [P.buf() for _ in range(4)]
        wds = [P.dsem() for _ in range(4)]
        P.dma("sp", lambda e: e.dma_start(out=wres[:, :, 0:512], in_=wv[:, :, 0:512]), wds[0], reads=wbufs, writes=[wres_b[0]])
        mT0, mT0_b, ml0, _ = m_r.next()
        m_r.i -= 1
        P.dma("sp", lambda e: e.dma_start(out=mT0[:], in_=kb.ap["mixT"][:, :, 0:512].rearrange("c p t -> p c t")), ml0, reads=[kb.db["mixT"][0]], writes=[mT0_b])
        first_m = dict(tb=0, mT=mT0, mT_b=mT0_b)
        for n in range(1, 4):
            P.dma("sp", lambda e: e.dma_start(out=wres[:, :, 512 * n:512 * (n + 1)], in_=wv[:, :, 512 * n:512 * (n + 1)]), wds[n], reads=wbufs, writes=[wres_b[n]])
        g, g_b = load_bcast(kb, ln_g, layer)
        bb, bb_b = load_bcast(kb, ln_b, layer)
        xb_r = Ring(kb, "xb", 2, [128, D], BF16)
        oT_r = Ring(kb, "oT", 1, [128, 16, 512], BF16)
        sm_r = Ring(kb, "sm", 4, [128, 32], F32)

        def load_m(tb):
            mT, mT_b, ml, _ = m_r.next()
            P.dma("sp", lambda e: e.dma_start(out=mT[:], in_=kb.ap["mixT"][:, :, 512 * tb:512 * (tb + 1)].rearrange("c p t -> p c t")), ml, reads=[kb.db["mixT"][tb]], writes=[mT_b])
            return dict(tb=tb, mT=mT, mT_b=mT_b)

        def load_x(c):
            tb = c["tb"]
            xr, _, xl, _ = xr_r.next()
            sl = xr_r.i % 2
            tbs = xr_tb[sl]
            for n in range(4):
                P.dma("pool", lambda e: e.dma_start(out=xr[:, :, 512 * n:512 * (n + 1)], in_=kb.ap[xres_name][512 * tb:512 * (tb + 1), 512 * n:512 * (n + 1)].rearrange("(t p) d -> p t d", p=128)),
                      xl, reads=[kb.db[xres_name][tb]], writes=[tbs[t][n] for t in range(4)])
            c.update(xr=xr, tbs=tbs, st=xr_r.st[sl])
            return c

        def epi_ln(c, t):
            r = c["xr"][:, t, :]
            ln_tile(kb, r, c["tbs"][t], g, g_b, bb, bb_b, sm_r)
            xb, xb_b = xb_r.next()[:2]
            ce = "act" if kb.alt() else "dve"
            if ce == "act":
                P.op("act", lambda e: e.copy(out=xb[:], in_=r), reads=c["tbs"][t], writes=[xb_b])
            else:
                P.op("dve", lambda e: e.tensor_copy(out=xb[:], in_=r), reads=c["tbs"][t], writes=[xb_b])
            c["xb%d" % t] = (xb, xb_b)
            tb = c["tb"]
            r0 = 512 * tb + 128 * t
            P.dma("act", lambda e: e.dma_start(out=kb.ap[out_name][r0:r0 + 128, :], in_=r), c["st"], reads=c["tbs"][t], writes=[kb.db[out_name][tb]])

        def epi_tr(c, t):
            if t == 0:
                c["oT"] = oT_r.next()
            oT, oT_b, _, ost = c["oT"]
            xb, xb_b = c["xb%d" % t]
            transpose_tile(kb, xb, xb_b, D, oT, oT_b, 128 * t, ident[:], ident_b)
            if t == 3:
                tb = c["tb"]
                P.dma("act", lambda e: e.dma_start(out=kb.ap[outT_name][:, :, 512 * tb:512 * (tb + 1)].rearrange("c p t -> p c t"), in_=oT[:]), ost, reads=[oT_b], writes=[kb.db[outT_name][tb]])

        m_r.i += 1
        nxt = load_x(first_m)
        pend = None
        for tb in range(NB):
            c = nxt
            mT, mT_b, xr, tbs = c["mT"], c["mT_b"], c["xr"], c["tbs"]
            pump(kb, n=4, gate=[mT_b])
            for n in range(4):
                wt, wb_ = wres[:, :, 512 * n:512 * (n + 1)], wres_b[n]
                if n == 1 and tb + 1 < NB:
                    nxt = load_m(tb + 1)
                if pend is not None and n in (0, 2):
                    epi_ln(pend, n)
                    epi_ln(pend, n + 1)
                for t in range(4):
                    ps, psb = kb.bank(2, 8)
                    for kc in range(16):
                        P.op("pe", lambda e, kc=kc: e.matmul(ps[:], lhsT=mT[:, kc, 128 * t:128 * (t + 1)], rhs=wt[:, kc, :], start=(kc == 0), stop=(kc == 15)),
                             reads=[mT_b, wb_], writes=[psb], pe_accum=True)
                    sl = xr[:, t, 512 * n:512 * (n + 1)]
                    P.op("dve", lambda e: e.scalar_tensor_tensor(out=sl, in0=sl, scalar=ALPHA, in1=ps[:], op0=ALU.mult, op1=ALU.add), reads=[psb, tbs[t][n]], writes=[tbs[t][n]])
                if pend is not None and n in (1, 3):
                    epi_tr(pend, n - 1)
                    epi_tr(pend, n)
                if n == 3 and tb + 1 < NB:
                    nxt = load_x(nxt)
            pend = c
        for t in range(4):
            epi_ln(pend, t)
            epi_tr(pend, t)


def phase_D(kb, layer, in_name, inT_name, out_name, outT_name):
    P = kb.P
    wu = kb.ap[f"wb_up{layer}"].rearrange("(kc p) n -> p kc n", p=128)
    wd = kb.ap[f"wb_down{layer}"].rearrange("(kc p) n -> p kc n", p=128)
    wu_b, wd_b = need_w(kb, f"wb_up{layer}"), need_w(kb, f"wb_down{layer}")
    with kb.phase():
        C = load_consts(kb)
        ident, ident_b = C["ident_bf"]
        xT_r = Ring(kb, "xT", 1, [128, 16, 512], BF16)
        hT = kb.sb("hT", [128, 64, 512], BF16)
        hT_b = [P.buf() for _ in range(64)]
        w_r = Ring(kb, "wst", 3, [128, 16, 512], BF16)
        xr_r = Ring(kb, "xr", 1, [128, 4, D], F32)
        tbs = [[P.buf() for _ in range(4)] for _ in range(4)]
        xb_r = Ring(kb, "xb", 2, [128, D], BF16)
        oT_r = Ring(kb, "oT", 1, [128, 16, 512], BF16)
        sm_r = Ring(kb, "sm", 4, [128, 32], F32)
        tmp_r = Ring(kb, "tmp", 2, [128, 512], F32)

        def load_xT(tb):
            xT, xT_b, xl, _ = xT_r.next()
            P.dma("sp", lambda e: e.dma_start(out=xT[:], in_=kb.ap[inT_name][:, :, 512 * tb:512 * (tb + 1)].rearrange("c p t -> p c t")), xl, reads=[kb.db[inT_name][tb]], writes=[xT_b])
            return xT, xT_b

        def load_x(tb):
            xr, _, xrl, xrs = xr_r.next()
            for n in range(4):
                P.dma("pool", lambda e: e.dma_start(out=xr[:, :, 512 * n:512 * (n + 1)], in_=kb.ap[in_name][512 * tb:512 * (tb + 1), 512 * n:512 * (n + 1)].rearrange("(t p) d -> p t d", p=128)),
                      xrl, reads=[kb.db[in_name][tb]], writes=[tbs[t][n] for t in range(4)])
            return xr, xrs

        def epi_ln(c, t):
            r = c["xr"][:, t, :]
            ln_tile(kb, r, tbs[t], g, g_b, bb, bb_b, sm_r)
            xb, xb_b = xb_r.next()[:2]
            if kb.alt():
                P.op("act", lambda e: e.copy(out=xb[:], in_=r), reads=tbs[t], writes=[xb_b])
            else:
                P.op("dve", lambda e: e.tensor_copy(out=xb[:], in_=r), reads=tbs[t], writes=[xb_b])
            c["xb%d" % t] = (xb, xb_b)
            tb = c["tb"]
            r0 = 512 * tb + 128 * t
            P.dma("act", lambda e: e.dma_start(out=kb.ap[out_name][r0:r0 + 128, :], in_=r), c["xrs"], reads=tbs[t], writes=[kb.db[out_name][tb]])

        def epi_tr(c, t):
            if t == 0:
                c["oT"] = oT_r.next()
            oT, oT_b, _, ost = c["oT"]
            xb, xb_b = c["xb%d" % t]
            transpose_tile(kb, xb, xb_b, D, oT, oT_b, 128 * t, ident[:], ident_b)
            if t == 3:
                tb = c["tb"]
                P.dma("act", lambda e: e.dma_start(out=kb.ap[outT_name][:, :, 512 * tb:512 * (tb + 1)].rearrange("c p t -> p c t"), in_=oT[:]), ost, reads=[oT_b], writes=[kb.db[outT_name][tb]])

        nxt_xT = load_xT(0)
        xr, xrs = load_x(0)
        g, g_b = load_bcast(kb, "ln_ffn_g", layer)
        bb, bb_b = load_bcast(kb, "ln_ffn_b", layer)
        pend = None
        for tb in range(NB):
            xT, xT_b = nxt_xT
            c = dict(tb=tb)
            pump(kb, n=12, gate=[xT_b])
            for n in range(16):
                wt, wb_, wl, _ = w_r.next()
                P.dma("sp", lambda e: e.dma_start(out=wt[:], in_=wu[:, :, 512 * n:512 * (n + 1)]), wl, reads=wu_b, writes=[wb_])
                if pend is not None and n < 4:
                    epi_ln(pend, n)
                for m in range(4):
                    ps, psb = kb.bank(2, 4)
                    for kc in range(16):
                        P.op("pe", lambda e, kc=kc: e.matmul(ps[:], lhsT=wt[:, kc, 128 * m:128 * (m + 1)], rhs=xT[:, kc, :], start=(kc == 0), stop=(kc == 15)),
                             reads=[xT_b, wb_], writes=[psb], pe_accum=True)
                    hc = 4 * n + m
                    tmp, tmp_b, _, _ = tmp_r.next()
                    if m % 2 == 0:
                        P.op("act", lambda e: e.activation(out=tmp[:], in_=ps[:], func=AF.Relu), reads=[psb], writes=[tmp_b])
                        P.op("pool", lambda e: e.tensor_tensor(out=hT[:, hc, :], in0=tmp[:], in1=tmp[:], op=ALU.mult), reads=[tmp_b], writes=[hT_b[hc]])
                    else:
                        P.op("dve", lambda e: e.tensor_scalar_max(out=tmp[:], in0=ps[:], scalar1=0.0), reads=[psb], writes=[tmp_b])
                        P.op("act", lambda e: e.activation(out=hT[:, hc, :], in_=tmp[:], func=AF.Square), reads=[tmp_b], writes=[hT_b[hc]])
                if pend is not None and 1 <= n < 5:
                    epi_tr(pend, n - 1)
                if pend is not None and n == 12:
                    xr, xrs = load_x(tb)
            c["xr"], c["xrs"] = xr, xrs
            for n in range(4):
                banks = [(kb.ps[4 + t], kb.pb[4 + t]) for t in range(4)]
                for pc in range(4):
                    wt, wb_, wl, _ = w_r.next()
                    P.dma("sp", lambda e: e.dma_start(out=wt[:], in_=wd[:, 16 * pc:16 * (pc + 1), 512 * n:512 * (n + 1)]), wl, reads=wd_b, writes=[wb_])
                    if n == 0 and pc == 2 and tb + 1 < NB:
                        nxt_xT = load_xT(tb + 1)
                    for t in range(4):
                        ps, psb = banks[t]
                        for kl in range(16):
                            kc = 16 * pc + kl
                            P.op("pe", lambda e, kc=kc, kl=kl: e.matmul(ps[:], lhsT=hT[:, kc, 128 * t:128 * (t + 1)], rhs=wt[:, kl, :], start=(kc == 0), stop=(kc == 63)),
                                 reads=[hT_b[kc], wb_], writes=[psb], pe_accum=True)
                for t in range(4):
                    ps, psb = banks[t]
                    sl = xr[:, t, 512 * n:512 * (n + 1)]
                    P.op("dve", lambda e: e.scalar_tensor_tensor(out=sl, in0=sl, scalar=ALPHA, in1=ps[:], op0=ALU.mult, op1=ALU.add), reads=[psb, tbs[t][n]], writes=[tbs[t][n]])
            pend = c
        for t in range(4):
            epi_ln(pend, t)
            epi_tr(pend, t)


def phase_E(kb, layer, in_name, inT_name, out_name):
    P = kb.P
    wg = kb.ap[f"wb_gate{layer}"].rearrange("(kc p) n -> p kc n", p=128)
    wg_b = need_w(kb, f"wb_gate{layer}")
    wp_d = kb.ap[f"wb_ple{layer}"].rearrange("(kc p) n -> p kc n", p=128)
    with kb.phase():
        C = load_consts(kb)
        ident, ident_b = C["ident_bf"]
        wp = kb.sb("wp", [128, 2, D], BF16)
        wp_b = P.buf()
        wple_bufs = need_w(kb, f"wb_ple{layer}")
        xT_r = Ring(kb, "xT", 2, [128, 16, 512], BF16)
        xr_r = Ring(kb, "xr", 2, [128, 4, D], F32)
        xr_tb = [[P.buf() for _ in range(4)] for _ in range(2)]
        wres = kb.sb("wres", [128, 16, D], BF16)
        wres_b = [P.buf() for _ in range(4)]
        wds = [P.dsem() for _ in range(4)]
        P.dma("sp", lambda e: e.dma_start(out=wres[:, :, 0:512], in_=wg[:, :, 0:512]), wds[0], reads=wg_b, writes=[wres_b[0]])
        ps_r = Ring(kb, "pst", 8, [128, 256], F32)
        xb_r = Ring(kb, "xb", 2, [128, 256], BF16)
        pT_r = Ring(kb, "pT", 2, [128, 2, 512], BF16)
        tmp_r = Ring(kb, "tmp", 3, [128, 512], F32)

        def load_blk(tb):
            xT, xT_b, xl, _ = xT_r.next()
            xr, _, xrl, _ = xr_r.next()
            tbs = xr_tb[xr_r.i % 2]
            P.dma("sp", lambda e: e.dma_start(out=xT[:], in_=kb.ap[inT_name][:, :, 512 * tb:512 * (tb + 1)].rearrange("c p t -> p c t")), xl, reads=[kb.db[inT_name][tb]], writes=[xT_b])
            P.dma("sp", lambda e: e.dma_start(out=xr[:], in_=kb.ap[in_name][512 * tb:512 * (tb + 1), :].rearrange("(t p) d -> p t d", p=128)), xrl, reads=[kb.db[in_name][tb]], writes=tbs)
            pT, pT_b, _, _ = pT_r.next()
            psts = []
            for t in range(4):
                pst, pst_b, pl, _ = ps_r.next()
                r0 = 512 * tb + 128 * t
                P.dma("sp", lambda e: e.dma_start(out=pst[:], in_=kb.ap["p"][layer, r0:r0 + 128, :]), pl, writes=[pst_b])
                psts.append((pst, pst_b))
            return [xT, xT_b, xr, tbs, pT, pT_b, psts]

        def p_transposes(blk):
            pT, pT_b, psts = blk[4], blk[5], blk[6]
            for t, (pst, pst_b) in enumerate(psts):
                cast_transpose(kb, pst[:], pst_b, 256, xb_r, pT, pT_b, 128 * t, ident[:], ident_b)

        nxt = load_blk(0)
        P.dma("sp", lambda e: e.dma_start(out=wp[:], in_=wp_d), P.dsem(), reads=wple_bufs, writes=[wp_b])
        for n in range(1, 4):
            P.dma("sp", lambda e: e.dma_start(out=wres[:, :, 512 * n:512 * (n + 1)], in_=wg[:, :, 512 * n:512 * (n + 1)]), wds[n], reads=wg_b, writes=[wres_b[n]])
        p_transposes(nxt)
        for tb in range(NB):
            xT, xT_b, xr, tbs, pT, pT_b, _ = nxt
            for n in range(4):
                wt, wb_ = wres[:, :, 512 * n:512 * (n + 1)], wres_b[n]
                if n == 1 and tb + 1 < NB:
                    nxt = load_blk(tb + 1)
                for t in range(4):
                    G, G_b = kb.bank(2, 5)
                    E, E_b = kb.bank(5, 8)
                    for kc in range(16):
                        P.op("pe", lambda e, kc=kc: e.matmul(G[:], lhsT=xT[:, kc, 128 * t:128 * (t + 1)], rhs=wt[:, kc, :], start=(kc == 0), stop=(kc == 15)),
                             reads=[xT_b, wb_], writes=[G_b], pe_accum=True)
                    for kc in range(2):
                        P.op("pe", lambda e, kc=kc: e.matmul(E[:], lhsT=pT[:, kc, 128 * t:128 * (t + 1)], rhs=wp[:, kc, 512 * n:512 * (n + 1)], start=(kc == 0), stop=(kc == 1)),
                             reads=[pT_b, wp_b], writes=[E_b], pe_accum=True)
                    tmp, tmp_b, _, _ = tmp_r.next()
                    sl = xr[:, t, 512 * n:512 * (n + 1)]
                    P.op("act", lambda e: e.activation(out=tmp[:], in_=G[:], func=AF.Sigmoid), reads=[G_b], writes=[tmp_b])
                    P.op("dve", lambda e: e.tensor_tensor(out=tmp[:], in0=tmp[:], in1=E[:], op=ALU.mult), reads=[tmp_b, E_b], writes=[tmp_b])
                    P.op("pool", lambda e: e.tensor_tensor(out=sl, in0=sl, in1=tmp[:], op=ALU.add), reads=[tmp_b, tbs[t]], writes=[tbs[t]])
                if n == 3 and tb + 1 < NB:
                    p_transposes(nxt)
            P.dma("act", lambda e: e.dma_start(out=kb.ap[out_name][512 * tb:512 * (tb + 1), :].rearrange("(t p) d -> p t d", p=128), in_=xr[:]), xr_r.st[xr_r.i % 2], reads=tbs, writes=[kb.db[out_name][tb]])


def build(n_layers=2, dbg=(), stop=None):
    kb = KB(dbg)
    nc, P = kb.nc, kb.P
    x = kb.din("x", (S, D))
    kb.db["x"] = [P.buf() for _ in range(NB)]
    kb.din("p", (2, S, 256))
    for nm, shp in [("ev_w_in", (1, D, EV_IN)), ("ev_b_fgate", (1, 8)), ("ev_dw_kernel", (1, 31, 1024)), ("ev_dw_bias", (1, 1024)),
                    ("ev_cnorm_g", (1, 1024)), ("ev_cnorm_b", (1, 1024)), ("ev_w_out", (1, D, D)), ("od_w_in", (1, D, OD_IN)),
                    ("od_b_igate", (1, 8)), ("od_b_fgate", (1, 8)), ("od_w_out", (1, D, D)), ("ln_mix_g", (2, D)), ("ln_mix_b", (2, D)),
                    ("w_up", (2, D, DFF)), ("w_down", (2, DFF, D)), ("ln_ffn_g", (2, D)), ("ln_ffn_b", (2, D)),
                    ("w_ple", (2, 256, D)), ("w_ple_gate", (2, D, D))]:
        kb.din(nm, shp)
    for nm, dt in (("ident_bf", BF16), ("tri_bf", BF16), ("ones_bf", BF16), ("tri_f", F32), ("ones_f", F32), ("ident_f", F32)):
        kb.din(nm, (128, 128), dt)
    kb.dscr("qT", (8, 128, S), BF16)
    kb.dscr("kT", (8, 128, S), BF16)
    kb.dscr("v", (S, D), BF16)
    kb.dscr("g", (8, 128, S), BF16)
    kb.dscr("gates", (S, 16), F32, nbuf=1)
    kb.dscr("oT", (16, 128, S), BF16)
    kb.dscr("co", (8, 128, S), F32)
    kb.dscr("mixT", (16, 128, S), BF16)
    kb.dscr("x1", (S, D), F32)
    kb.dscr("x1T", (16, 128, S), BF16)
    kb.dscr("x2", (S, D), F32)
    kb.dscr("x2T", (16, 128, S), BF16)
    kb.dscr("xn", (S, D), F32)
    kb.dscr("y", (S, D), F32, out=True)

    def conv_layer(l):
        if l == 0:
            convert_weight(kb, kb.ap["ev_w_in"][0], "wb_ev_in", (D, EV_IN))
            convert_weight(kb, kb.ap["ev_w_out"][0], "wb_ev_out", (D, D))
        else:
            convert_weight(kb, kb.ap["od_w_in"][0], "wb_od_in", (D, OD_IN))
            convert_weight(kb, kb.ap["od_w_out"][0], "wb_od_out", (D, D))
        convert_weight(kb, kb.ap["w_up"][l], f"wb_up{l}", (D, DFF))
        convert_weight(kb, kb.ap["w_down"][l], f"wb_down{l}", (DFF, D))
        convert_weight(kb, kb.ap["w_ple_gate"][l], f"wb_gate{l}", (D, D))
        convert_weight(kb, kb.ap["w_ple"][l], f"wb_ple{l}", (256, D))

    conv_layer(0)
    steps = []
    for l in range(n_layers):
        last_l = (l == n_layers - 1)
        xin = "x" if l == 0 else "xn"
        if l == 0:
            steps.append(("A0", lambda: phase_A(kb, 0, (kb.ap["x"], kb.db["x"]))))
            if n_layers > 1:
                steps.append(("cv1", lambda: conv_layer(1)))
            steps.append(("B0", lambda: phase_B0(kb)))
            steps.append(("B2", lambda: phase_B2(kb)))
            steps.append(("C0", lambda: phase_C(kb, 0, "x", "wb_ev_out", "ln_mix_g", "ln_mix_b", "x1", "x1T")))
        else:
            steps.append(("A1", lambda: phase_A(kb, 1, (kb.ap["xn"], kb.db["xn"]))))
            steps.append(("B1", lambda: phase_B1(kb)))
            steps.append(("C1", lambda: phase_C(kb, 1, "xn", "wb_od_out", "ln_mix_g", "ln_mix_b", "x1", "x1T")))
        steps.append((f"D{l}", lambda l=l: phase_D(kb, l, "x1", "x1T", "x2", "x2T")))
        steps.append((f"E{l}", lambda l=l, last_l=last_l: phase_E(kb, l, "x2", "x2T", "y" if last_l else "xn")))
    for nm, fn in steps:
        fn()
        if stop == nm:
            break
    finals = list(P.pending_dma)
    if not finals:
        finals = [o for o in P.last.values() if o is not None]
    P.emit(nc, final_wait_ops=finals)
    return kb


def consts():
    i = np.eye(128, dtype=np.float32)
    tri = np.triu(np.ones((128, 128), np.float32))
    one = np.ones((128, 128), np.float32)
    bf = ml_dtypes.bfloat16
    return {"ident_bf": i.astype(bf), "tri_bf": tri.astype(bf), "ones_bf": one.astype(bf), "tri_f": tri, "ones_f": one, "ident_f": i}


_KB = None


def kernel(**inputs):
    global _KB
    if _KB is None:
        _KB = build(n_layers=2)
    kb = _KB
    c = consts()
    x = np.asarray(inputs["x"], dtype=np.float32)
    p = np.asarray(inputs["p"], dtype=np.float32)
    shared = {k: np.ascontiguousarray(np.asarray(v, dtype=np.float32)) for k, v in inputs.items() if k not in ("x", "p")}
    in_maps = []
    for b in range(8):
        m = dict(c)
        m.update(shared)
        m["x"] = np.ascontiguousarray(x[b])
        m["p"] = np.ascontiguousarray(p[:, b])
        in_maps.append(m)
    res = run_bass_kernel_spmd(kb.nc, in_maps, core_ids=list(range(8)))
    return np.stack([np.asarray(r["y"], dtype=np.float32) for r in res.results], axis=0)
```
